# Optimizing a Trainium2 kernel written in Bass

```python
import math
import jax, jax.numpy as jnp
from jax import lax
import numpy as np

D_MODEL = 2048
BATCH = 4
SEQ = 2048
DEPTH = 1

N_META = 16
MLA_HEADS = 8
QK_NOPE_DIM = 128
QK_ROPE_DIM = 64
QK_HEAD_DIM = QK_NOPE_DIM + QK_ROPE_DIM
V_HEAD_DIM = 128
Q_LORA_RANK = 768
KV_LORA_RANK = 512
ROPE_THETA = 10000.0
Q_BLOCK = 128
MLA_WIDTH = MLA_HEADS * V_HEAD_DIM
CONV_CHANNELS = D_MODEL // 2
CONV_WIDTH = 31
MIX_WIDTH = MLA_WIDTH + CONV_CHANNELS
IN_PROJ_WIDTH = Q_LORA_RANK + KV_LORA_RANK + QK_ROPE_DIM + 2 * CONV_CHANNELS
N_EXPERTS = 32
TOP_K = 4
D_FF = D_MODEL
SWIGLU_LIMIT = 7.0
SWIGLU_ALPHA = 1.702
EXPERT_BLOCK = 128
DEEPNORM_ALPHA = (2.0 * DEPTH) ** 0.25
DEEPNORM_BETA = (8.0 * DEPTH) ** -0.25
LN_EPS = 1e-5
RMS_EPS = 1e-6

kernel_name = "hymba_mla_conformer_moe_deepnorm"


def layer_norm(x, g, b):
    xf = x.astype(jnp.float32)
    mu = jnp.mean(xf, axis=-1, keepdims=True)
    var = jnp.mean(jnp.square(xf - mu), axis=-1, keepdims=True)
    return ((xf - mu) * lax.rsqrt(var + LN_EPS)).astype(x.dtype) * g + b


def rms_norm(x, g):
    xf = x.astype(jnp.float32)
    ms = jnp.mean(jnp.square(xf), axis=-1, keepdims=True)
    return (xf * lax.rsqrt(ms + RMS_EPS)).astype(x.dtype) * g


def rope_tables(length, dtype):
    inv_freq = 1.0 / (ROPE_THETA ** (jnp.arange(0, QK_ROPE_DIM, 2, dtype=jnp.float32) / QK_ROPE_DIM))
    pos = jnp.arange(length, dtype=jnp.float32)
    freqs = pos[:, None] * inv_freq[None, :]
    emb = jnp.concatenate([freqs, freqs], axis=-1)
    return jnp.cos(emb).astype(dtype), jnp.sin(emb).astype(dtype)


def apply_rope(x, cos, sin):
    half = QK_ROPE_DIM // 2
    x1, x2 = x[..., :half], x[..., half:]
    rot = jnp.concatenate([-x2, x1], axis=-1)
    return x * cos + rot * sin


def mla_group(c_q, c_kv, k_pe, q_norm_g, w_uq, kv_norm_g, w_uk, w_uv, cos, sin):
    B, L, _ = c_q.shape
    q = (rms_norm(c_q, q_norm_g) @ w_uq).reshape(B, L, MLA_HEADS, QK_HEAD_DIM)
    q_nope, q_pe = q[..., :QK_NOPE_DIM], q[..., QK_NOPE_DIM:]
    q_pe = apply_rope(q_pe, cos[:, None, :], sin[:, None, :])
    ckv = rms_norm(c_kv, kv_norm_g)
    k_nope = (ckv @ w_uk).reshape(B, L, MLA_HEADS, QK_NOPE_DIM)
    v = (ckv @ w_uv).reshape(B, L, MLA_HEADS, V_HEAD_DIM)
    k_pe = apply_rope(k_pe, cos, sin)
    q = jnp.concatenate([q_nope, q_pe], axis=-1)
    k = jnp.concatenate([k_nope, jnp.broadcast_to(k_pe[:, :, None, :], (B, L, MLA_HEADS, QK_ROPE_DIM))], axis=-1)

    n_blocks = -(-L // Q_BLOCK)
    L_pad = n_blocks * Q_BLOCK
    pad = [(0, 0), (0, L_pad - L), (0, 0), (0, 0)]
    q, k, v = jnp.pad(q, pad), jnp.pad(k, pad), jnp.pad(v, pad)
    q_blocks = q.reshape(B, n_blocks, Q_BLOCK, MLA_HEADS, QK_HEAD_DIM).transpose(1, 0, 2, 3, 4)
    k_pos = jnp.arange(L_pad)
    scale = 1.0 / math.sqrt(QK_HEAD_DIM)

    def attend(args):
        qb, start = args
        s = jnp.einsum('bqhd,bkhd->bhqk', qb, k).astype(jnp.float32) * scale
        q_pos = start + jnp.arange(Q_BLOCK)
        mask = k_pos[None, :] <= q_pos[:, None]
        s = jnp.where(mask[None, None], s, -1e30)
        p = jax.nn.softmax(s, axis=-1).astype(v.dtype)
        return jnp.einsum('bhqk,bkhd->bqhd', p, v)

    out = lax.map(attend, (q_blocks, jnp.arange(n_blocks) * Q_BLOCK))
    out = out.transpose(1, 0, 2, 3, 4).reshape(B, L_pad, MLA_WIDTH)
    return out[:, :L]


def conv_group(u, dw_w, dw_b, ln_g, ln_b):
    a, gate = u[..., :CONV_CHANNELS], u[..., CONV_CHANNELS:]
    h = a * jax.nn.sigmoid(gate)
    h = lax.conv_general_dilated(
        h, dw_w[:, None, :], window_strides=(1,), padding=[(CONV_WIDTH - 1, 0)],
        dimension_numbers=('NWC', 'WIO', 'NWC'), feature_group_count=CONV_CHANNELS) + dw_b
    h = layer_norm(h, ln_g, ln_b)
    return jax.nn.silu(h)


def moe(h, w_router, b_router, w1, b1, w2, b2):
    B, L, D = h.shape
    xt = h.reshape(-1, D)
    T = xt.shape[0]
    logits = (xt @ w_router + b_router).astype(jnp.float32)
    top_vals, top_idx = lax.top_k(logits, TOP_K)
    gates = jax.nn.softmax(top_vals, axis=-1)

    TK = T * TOP_K
    expert_flat = top_idx.reshape(-1).astype(jnp.int32)
    token_flat = (jnp.arange(TK, dtype=jnp.int32) // TOP_K)
    gate_flat = gates.reshape(-1)
    order = jnp.argsort(expert_flat)
    e_sorted = expert_flat[order]
    counts = jnp.bincount(expert_flat, length=N_EXPERTS)
    starts = jnp.cumsum(counts) - counts
    padded = (counts + EXPERT_BLOCK - 1) // EXPERT_BLOCK * EXPERT_BLOCK
    pad_ends = jnp.cumsum(padded)
    pad_starts = pad_ends - padded
    dest = pad_starts[e_sorted] + (jnp.arange(TK) - starts[e_sorted])

    n_blocks = -(-TK // EXPERT_BLOCK) + N_EXPERTS
    n_rows = n_blocks * EXPERT_BLOCK
    row_token = jnp.zeros((n_rows,), jnp.int32).at[dest].set(token_flat[order])
    row_gate = jnp.zeros((n_rows,), jnp.float32).at[dest].set(gate_flat[order])
    block_expert = jnp.minimum(
        jnp.searchsorted(pad_ends, jnp.arange(n_blocks) * EXPERT_BLOCK, side='right'), N_EXPERTS - 1)

    def expert_block(args):
        tok, e = args
        xb = xt[tok]
        z = xb @ w1[e] + b1[e]
        g, u = z[:, :D_FF], z[:, D_FF:]
        g = jnp.minimum(g, SWIGLU_LIMIT)
        u = jnp.clip(u, -SWIGLU_LIMIT, SWIGLU_LIMIT)
        act = g * jax.nn.sigmoid(SWIGLU_ALPHA * g) * (u + 1.0)
        return act @ w2[e] + b2[e]

    out = lax.map(expert_block, (row_token.reshape(n_blocks, EXPERT_BLOCK), block_expert))
    out = out.reshape(n_rows, D) * row_gate[:, None].astype(out.dtype)
    y = jnp.zeros_like(xt).at[row_token].add(out)
    return y.reshape(B, L, D)


def setup_inputs(seed: int = 0) -> dict:
    key = jax.random.key(seed)
    ks = jax.random.split(key, 32)
    f32 = jnp.float32

    def nrm(k, shape, fan_in, scale=1.0):
        return jax.random.normal(k, shape, f32) * (scale * fan_in ** -0.5)

    def gain(k, shape):
        return 1.0 + 0.02 * jax.random.normal(k, shape, f32)

    def bias(k, shape, s=0.02):
        return s * jax.random.normal(k, shape, f32)

    Dp = DEPTH
    return {
        "x": jax.random.normal(ks[0], (BATCH, SEQ, D_MODEL), f32),
        "meta_tokens": jax.random.normal(ks[1], (N_META, D_MODEL), f32),
        "ln_in_g": gain(ks[2], (D_MODEL,)),
        "ln_in_b": bias(ks[3], (D_MODEL,)),
        "w_in": nrm(ks[4], (Dp, D_MODEL, IN_PROJ_WIDTH), D_MODEL),
        "q_norm_g": gain(ks[5], (Dp, Q_LORA_RANK)),
        "w_uq": nrm(ks[6], (Dp, Q_LORA_RANK, MLA_HEADS * QK_HEAD_DIM), Q_LORA_RANK),
        "kv_norm_g": gain(ks[7], (Dp, KV_LORA_RANK)),
        "w_uk": nrm(ks[8], (Dp, KV_LORA_RANK, MLA_HEADS * QK_NOPE_DIM), KV_LORA_RANK),
        "w_uv": nrm(ks[9], (Dp, KV_LORA_RANK, MLA_HEADS * V_HEAD_DIM), KV_LORA_RANK, DEEPNORM_BETA),
        "conv_dw_w": nrm(ks[10], (Dp, CONV_WIDTH, CONV_CHANNELS), CONV_WIDTH),
        "conv_dw_b": bias(ks[11], (Dp, CONV_CHANNELS)),
        "conv_ln_g": gain(ks[12], (Dp, CONV_CHANNELS)),
        "conv_ln_b": bias(ks[13], (Dp, CONV_CHANNELS)),
        "w_out": nrm(ks[14], (Dp, MIX_WIDTH, D_MODEL), MIX_WIDTH, DEEPNORM_BETA),
        "ln1_g": gain(ks[15], (Dp, D_MODEL)),
        "ln1_b": bias(ks[16], (Dp, D_MODEL)),
        "w_router": nrm(ks[17], (Dp, D_MODEL, N_EXPERTS), D_MODEL),
        "b_router": bias(ks[18], (Dp, N_EXPERTS), 0.01),
        "w_mlp1": nrm(ks[19], (Dp, N_EXPERTS, D_MODEL, 2 * D_FF), D_MODEL),
        "b_mlp1": bias(ks[20], (Dp, N_EXPERTS, 2 * D_FF)),
        "w_mlp2": nrm(ks[21], (Dp, N_EXPERTS, D_FF, D_MODEL), D_FF, DEEPNORM_BETA),
        "b_mlp2": bias(ks[22], (Dp, N_EXPERTS, D_MODEL)),
        "ln2_g": gain(ks[23], (Dp, D_MODEL)),
        "ln2_b": bias(ks[24], (Dp, D_MODEL)),
    }


def reference(x, meta_tokens, ln_in_g, ln_in_b, w_in, q_norm_g, w_uq, kv_norm_g, w_uk, w_uv,
              conv_dw_w, conv_dw_b, conv_ln_g, conv_ln_b, w_out, ln1_g, ln1_b,
              w_router, b_router, w_mlp1, b_mlp1, w_mlp2, b_mlp2, ln2_g, ln2_b):
    B = x.shape[0]
    meta = jnp.broadcast_to(meta_tokens[None].astype(x.dtype), (B, N_META, D_MODEL))
    h = jnp.concatenate([meta, x], axis=1)
    h = layer_norm(h, ln_in_g, ln_in_b)
    L = h.shape[1]
    cos, sin = rope_tables(L, h.dtype)
    s1 = Q_LORA_RANK
    s2 = s1 + KV_LORA_RANK
    s3 = s2 + QK_ROPE_DIM
    for l in range(DEPTH):
        proj = h @ w_in[l]
        c_q, c_kv, k_pe, u_conv = proj[..., :s1], proj[..., s1:s2], proj[..., s2:s3], proj[..., s3:]
        attn = mla_group(c_q, c_kv, k_pe, q_norm_g[l], w_uq[l], kv_norm_g[l], w_uk[l], w_uv[l], cos, sin)
        conv = conv_group(u_conv, conv_dw_w[l], conv_dw_b[l], conv_ln_g[l], conv_ln_b[l])
        mix = jnp.concatenate([attn, conv], axis=-1) @ w_out[l]
        h = layer_norm(DEEPNORM_ALPHA * h + mix, ln1_g[l], ln1_b[l])
        ffn = moe(h, w_router[l], b_router[l], w_mlp1[l], b_mlp1[l], w_mlp2[l], b_mlp2[l])
        h = layer_norm(DEEPNORM_ALPHA * h + ffn, ln2_g[l], ln2_b[l])
    return h[:, N_META:]
```

```python
import math
from contextlib import ExitStack
import numpy as np
import concourse.bass as bass
import concourse.mybir as mybir
from concourse.bass_utils import run_bass_kernel_spmd

F32 = mybir.dt.float32
BF16 = mybir.dt.bfloat16
AF = mybir.ActivationFunctionType
ALU = mybir.AluOpType

D = 2048
SEQ = 2048
NMETA = 16
LK = SEQ + NMETA
LKP = SEQ + 128
NOWN = 1024
NE = 32
CAP = 256
DFF = 2048
ALPHA = 2.0 ** 0.25
LN_EPS = 1e-5
RMS_EPS = 1e-6
QSCALE = 1.0 / math.sqrt(192.0)
NEG = -30000.0
CELL = 32
SB_LIMIT = 212000
SB_BASE = 16640

ENGS = ['pe', 'act', 'dve', 'pool', 'sp']


def _is_ap(x):
    return hasattr(x, 'ap') and hasattr(x, 'tensor') and hasattr(x, 'offset')


class Prog:
    def __init__(self, nc):
        self.nc = nc
        self.ops = []
        self.vbase = {}
        ncell = (SB_LIMIT + 8 * 2048 + 4096) // CELL + 8
        self.last_w = np.full(ncell, -1, np.int64)
        self.last_r = {e: np.full(ncell, -1, np.int64) for e in ENGS}
        self.cache = {}
        self.dma_keys = {}
        self.last_dma = {}

    def reg(self, handle, vbase):
        self.vbase[handle.name] = vbase

    def cells(self, ap):
        sp = str(ap.space)
        if 'DRAM' in sp.upper():
            return None
        key = (ap.tensor.name, int(ap.offset), tuple(tuple(x) for x in ap.ap), str(ap.dtype))
        c = self.cache.get(key)
        if c is not None:
            return c
        es = mybir.dt.size(ap.dtype)
        dims = [tuple(x) for x in ap.ap]
        pstep = dims[0][0]
        off = int(ap.offset) % pstep if pstep > 0 else int(ap.offset)
        base = self.vbase[ap.tensor.name]
        idx = np.array([off], dtype=np.int64)
        free = dims[1:]
        if len(free) == 0:
            free = [(1, 1)]
        for step, cnt in free[:-1]:
            idx = (idx[:, None] + (np.arange(cnt, dtype=np.int64) * step)[None, :]).ravel()
        step, cnt = free[-1]
        if step == 1 or cnt == 1:
            lo = base + idx * es
            hi = base + (idx + cnt) * es - 1
        else:
            idx = (idx[:, None] + (np.arange(cnt, dtype=np.int64) * step)[None, :]).ravel()
            lo = base + idx * es
            hi = lo + es - 1
        lo = lo // CELL
        hi = hi // CELL
        n = int((hi - lo).max()) + 1
        c = np.unique((lo[:, None] + np.arange(n)[None, :]).clip(max=hi[:, None]))
        self.cache[key] = c
        return c

    def op(self, eng, fn, r=(), w=(), dma=None, ndma=0):
        i = len(self.ops)
        deps = set()
        rcells = [c for c in (self.cells(a) for a in r) if c is not None]
        wcells = [c for c in (self.cells(a) for a in w) if c is not None]
        raw = set()
        for c in rcells:
            u = np.unique(self.last_w[c])
            raw.update(int(x) for x in u if x >= 0)
        deps |= raw
        for c in wcells:
            u = np.unique(self.last_w[c])
            deps.update(int(x) for x in u if x >= 0)
            for e in ENGS:
                u = np.unique(self.last_r[e][c])
                deps.update(int(x) for x in u if x >= 0)
        keep = set()
        for d in deps:
            od = self.ops[d]
            if od['eng'] == eng and od['dma'] is None and dma is None:
                if eng == 'pe':
                    continue
                if d not in raw:
                    continue
            keep.add(d)
        if dma is not None and dma in self.last_dma:
            keep.add(self.last_dma[dma])
        for c in rcells:
            self.last_r[eng][c] = i
        for c in wcells:
            self.last_w[c] = i
            for e in ENGS:
                self.last_r[e][c] = -1
        self.ops.append(dict(eng=eng, fn=fn, deps=keep, dma=dma, ndma=ndma, signal=False))
        if dma is not None:
            self.last_dma[dma] = i
        return i

    def selfcheck(self, by_eng):
        ops = self.ops
        sem = {}
        pc = {e: 0 for e in ENGS}
        progress = True
        while progress:
            progress = False
            for e in ENGS:
                lst = by_eng[e]
                while pc[e] < len(lst):
                    o = lst[pc[e]]
                    ok = True
                    for d in o['deps']:
                        if 'sig' not in ops[d]:
                            raise RuntimeError("dep on unsignaled op %d" % d)
                        sn, val = ops[d]['sig']
                        if sem.get(sn, 0) < val:
                            ok = False
                            break
                    if not ok:
                        break
                    if o['dma'] is not None:
                        sn = 'd:' + o['dma']
                        sem[sn] = sem.get(sn, 0) + 16 * o['ndma']
                    elif o['signal']:
                        sn = 'e:' + o['eng']
                        sem[sn] = sem.get(sn, 0) + 1
                    pc[e] += 1
                    progress = True
        for e in ENGS:
            if pc[e] < len(by_eng[e]):
                o = by_eng[e][pc[e]]
                raise RuntimeError("deadlock: engine %s stuck at op %d/%d deps=%s" % (e, pc[e], len(by_eng[e]), [(d, ops[d]['eng'], ops[d].get('sig')) for d in o['deps']]))

    def emit(self):
        nc = self.nc
        ops = self.ops
        for o in ops:
            for d in o['deps']:
                ops[d]['signal'] = True
        cnt = {e: 0 for e in ENGS}
        dcnt = {}
        for o in ops:
            if o['dma'] is not None:
                k = o['dma']
                dcnt[k] = dcnt.get(k, 0) + 16 * o['ndma']
                o['sig'] = ('d:' + k, dcnt[k])
            elif o['signal']:
                cnt[o['eng']] += 1
                o['sig'] = ('e:' + o['eng'], cnt[o['eng']])
        semnames = ['e:' + e for e in ENGS] + ['d:' + k for k in dcnt]
        print('PROG tickets', cnt, 'dma', {k: v for k, v in dcnt.items() if v > 1000}, 'nops', len(ops), flush=True)
        by_eng = {e: [o for o in ops if o['eng'] == e] for e in ENGS}
        self.selfcheck(by_eng)
        with ExitStack() as st:
            sems = {}
            for j, n in enumerate(semnames):
                sems[n] = st.enter_context(nc.semaphore("s%d" % j))
            block = st.enter_context(nc.Block())

            def run(e, lst):
                waited = {}
                for o in lst:
                    need = {}
                    for d in o['deps']:
                        sn, val = ops[d]['sig']
                        if need.get(sn, 0) < val:
                            need[sn] = val
                    for sn, val in need.items():
                        if waited.get(sn, 0) < val:
                            e.wait_ge(sems[sn], val)
                            waited[sn] = val
                    if o['dma'] is not None:
                        o['fn'](e, sems['d:' + o['dma']])
                    else:
                        ins = o['fn'](e)
                        if o['signal']:
                            ins.then_inc(sems['e:' + o['eng']], 1)

            @block.tensor
            def _(e):
                run(e, by_eng['pe'])

            @block.scalar
            def _(e):
                run(e, by_eng['act'])

            @block.vector
            def _(e):
                run(e, by_eng['dve'])

            @block.gpsimd
            def _(e):
                run(e, by_eng['pool'])

            @block.sync
            def _(e):
                run(e, by_eng['sp'])


def build_nc(stop=99, debug=False, ne_alloc=NE):
    nc = bass.Bass("TRN2", target_bir_lowering=False)
    P = Prog(nc)

    def din(name, shape, dt=F32):
        return nc.dram_tensor(name, list(shape), dt, kind="ExternalInput").ap()

    _specs = {
        "xall": ("xall", [2048 + 128, D]),
        "xhalo": ("xhalo", [128, D]),
        "halomask": ("halomask", [128, 64]),
        "cosT_d": ("cosT", [64, LKP]),
        "sinT_d": ("sinT", [64, LKP]),
        "kbias_d": ("kbias", [2, LKP]),
        "ident_d": ("ident", [128, 128]),
        "tri_d": ("tri", [128, 128]),
        "ustr_d": ("ustrict", [128, 128]),
        "iota_d": ("iota", [128, CAP]),
        "w_in": ("w_in", [D, 3392]),
        "w_uq": ("w_uq", [768, 1536]),
        "w_uk": ("w_uk", [512, 1024]),
        "w_uv": ("w_uv", [512, 1024]),
        "w_out": ("w_out", [D, D]),
        "w_router": ("w_router", [D, NE]),
        "w1": ("w1", [ne_alloc, D, 2 * DFF]),
        "w2": ("w2", [ne_alloc, DFF, D]),
        "lnin_cols_d": ("lnin_cols", [128, 32]),
        "lnin_bc_d": ("lnin_bc", [2, 128, D]),
        "ln1_bc_d": ("ln1_bc", [2, 128, D]),
        "ln2_bc_d": ("ln2_bc", [2, 128, D]),
        "qg_col_d": ("qg_col", [128, 6]),
        "kvg_col_d": ("kvg_col", [128, 4]),
        "dw_cols_d": ("dw_cols", [128, 8, 31]),
        "dwb_col_d": ("dwb_col", [128, 8]),
        "cln_cols_d": ("cln_cols", [128, 16]),
        "b1_cols_d": ("b1_cols", [128, NE, 32]),
        "b2_d": ("b2", [NE, D]),
        "brouter_d": ("brouter_bc", [128, NE]),
    }
    used_inputs = []

    class _T:
        def __init__(self):
            self.c = {}

        def __getattr__(self, k):
            c = self.__dict__['c']
            if k not in c:
                n, shp = _specs[k]
                c[k] = din(n, shp)
                used_inputs.append(n)
            return c[k]

    T = _T()
    out_d = nc.dram_tensor("out", [NOWN, D], F32, kind="ExternalOutput").ap()
    dbg = {}

    def sb(name, shape, dt, at):
        nbytes = int(np.prod(shape[1:])) * mybir.dt.size(dt)
        assert at % 32 == 0, (name, at)
        assert at + nbytes <= SB_LIMIT, (name, at, nbytes)
        h = nc.alloc_sbuf_tensor_at(name, list(shape), dt, offset=at + SB_BASE)
        P.reg(h, at)
        return h

    class Bump:
        def __init__(self, lo, hi):
            self.p = lo
            self.hi = hi

        def __call__(self, name, shape, dt):
            nbytes = int(np.prod(shape[1:])) * mybir.dt.size(dt)
            nbytes = (nbytes + 31) // 32 * 32
            at = self.p
            assert at + nbytes <= self.hi, (name, at, nbytes, self.hi)
            self.p += nbytes
            return sb(name, shape, dt, at)

    KB = 1024
    ps = []
    for i in range(8):
        h = nc.alloc_psum_tensor("ps%d" % i, [128, 512], F32)
        P.reg(h, SB_LIMIT + 2048 * i)
        ps.append(h)

    def I(eng, name, *args, **kw):
        w = [args[0]]
        xr = kw.pop('xr', [])
        r = [a for a in args[1:] if _is_ap(a)] + [v for k, v in kw.items() if _is_ap(v) and k != 'accum_out'] + list(xr)
        if 'accum_out' in kw:
            w.append(kw['accum_out'])
        return P.op(eng, lambda e: getattr(e, name)(*args, **kw), r=r, w=w)

    def mm(out, lhsT, rhs, start=True, stop=True):
        return P.op('pe', lambda e: e.matmul(out, lhsT, rhs, start=start, stop=stop, skip_group_check=True),
                    r=[lhsT, rhs], w=[out])

    def tp(out, in_, ident):
        return P.op('pe', lambda e: e.transpose(out, in_, ident), r=[in_, ident], w=[out])

    def dma(eng, out, in_, key):
        return dmas(eng, [(out, in_)], key)

    import os as _os
    _skip = set(_os.environ.get("KSKIP", "").split(","))

    def dmas(eng, pairs, key):
        if key in ("c0", "c1") and any(n and str(pairs[0][0].tensor.name).startswith(n) for n in _skip):
            return None
        def fn(e, sem):
            for o, i_ in pairs:
                e.dma_start(out=o, in_=i_).then_inc(sem, 16)
        r = [i_ for _, i_ in pairs]
        w = [o for o, _ in pairs]
        return P.op(eng, fn, r=r, w=w, dma=key, ndma=len(pairs))

    def act(out, in_, func, **kw):
        return I('act', 'activation', out, in_, func, **kw)

    kb = Bump(0, 18 * KB)
    ident = kb("ident", [128, 128], F32)
    ident_b = kb("ident_b", [128, 128], BF16)
    ones_b = kb("ones_b", [128, 128], BF16)
    ones_f = kb("ones_f", [128, 128], F32)
    ustr_b = kb("ustr_b", [128, 128], BF16)
    tri_b = kb("tri_b", [128, 128], BF16)
    iota_j = kb("iota_j", [128, CAP], F32)
    lnin_cols = kb("lnin_cols", [128, 32], F32)
    qg_col = kb("qg_col", [128, 6], F32)
    kvg_col = kb("kvg_col", [128, 4], F32)
    dw_cols = kb("dw_cols", [128, 8, 31], F32)
    dwb_col = kb("dwb_col", [128, 8], F32)
    cln_cols = kb("cln_cols", [128, 16], F32)
    b1_cols = kb("b1_cols", [128, NE, 32], F32)
    wr_sb = kb("wr_sb", [128, 16, NE], F32)
    brouter = kb("brouter", [128, NE], F32)
    hmask = kb("hmask", [128, 64], F32)
    rstd_col = kb("rstd_col", [128, 17], F32)
    Gt = kb("Gt", [128, 8, NE], F32)
    Ghl = kb("Ghl", [128, 8, NE, 2], BF16)
    posm = kb("posm", [128, 8, NE], F32)
    maskb = kb("maskb", [128, 8, NE], BF16)
    smallf = kb("smallf", [128, 256], F32)
    gate_g = kb("gate_g", [128, 2, 2], F32)
    GT_sb = kb("GT_sb", [128, 128], F32)

    dma('sp', ident[:], T.ident_d, 'c0')
    dma('pool', ident_b[:], T.ident_d, 'c1')
    dma('pool', ustr_b[:], T.ustr_d, 'c1')
    dma('pool', tri_b[:], T.tri_d, 'c1')
    dma('sp', iota_j[:], T.iota_d, 'c0')
    dma('sp', lnin_cols[:], T.lnin_cols_d, 'c0')
    dma('sp', qg_col[:], T.qg_col_d, 'c0')
    dma('sp', kvg_col[:], T.kvg_col_d, 'c0')
    dma('sp', dw_cols[:], T.dw_cols_d, 'c0')
    dma('sp', dwb_col[:], T.dwb_col_d, 'c0')
    dma('sp', cln_cols[:], T.cln_cols_d, 'c0')
    dma('sp', b1_cols[:], T.b1_cols_d, 'c0')
    dma('sp', wr_sb[:], T.w_router.rearrange("(kc p) n -> p kc n", p=128), 'c0')
    dma('sp', brouter[:], T.brouter_d, 'c0')
    dma('sp', hmask[:], T.halomask, 'c0')
    I('dve', 'memset', ones_b[:], 1.0)
    I('dve', 'memset', ones_f[:], 1.0)

    p2 = Bump(18 * KB, 85 * KB)
    ckvgT = p2("ckvgT", [128, 4, LKP], BF16)
    KpT = p2("KpT", [128, 2, LKP], BF16)
    cqgT = p2("cqgT", [128, 6, NOWN], BF16)
    rstd_kv = p2("rstd_kv", [128, 2048 + 128], F32)
    rstd_q = p2("rstd_q", [128, NOWN], F32)
    cosT = p2("cosT_sb", [64, LKP], F32)
    sinT = p2("sinT_sb", [64, LKP], F32)
    attnT = sb("attnT", [128, 8, NOWN], BF16, 175 * KB)
    convT = sb("convT", [128, 8, NOWN], BF16, 191 * KB)

    dma('sp', cosT[:], T.cosT_d, 'c0')
    dma('sp', sinT[:], T.sinT_d, 'c0')
    dma('pool', KpT[64:65, :, :], T.kbias_d.rearrange("(o s) n -> o s n", o=1), 'c1')

    stat_ctr = [0]

    def ln_stats(x_ap, rows, eps):
        k = stat_ctr[0] % 4
        stat_ctr[0] += 1
        base = k * 40
        st6 = smallf[:rows, base:base + 24]
        for j in range(4):
            I('dve', 'bn_stats', smallf[:rows, base + 6 * j: base + 6 * j + 6], x_ap[:, j * 512:(j + 1) * 512])
        mv = smallf[:rows, base + 24: base + 26]
        I('dve', 'bn_aggr', mv, st6)
        rstd = smallf[:rows, base + 26: base + 27]
        nmr = smallf[:rows, base + 27: base + 28]
        I('dve', 'tensor_scalar', rstd, smallf[:rows, base + 25: base + 26], eps, None, ALU.add)
        act(rstd, rstd, AF.Sqrt)
        I('dve', 'reciprocal', rstd, rstd)
        I('dve', 'scalar_tensor_tensor', nmr, smallf[:rows, base + 24: base + 25], -1.0, rstd, ALU.mult, ALU.mult)
        return rstd, nmr

    hT_b = [None] * 4
    hT_b[0] = sb("hT_b0", [128, 16, 512], BF16, 85 * KB)
    hT_b[3] = sb("hT_b3", [128, 16, 512], BF16, 101 * KB)
    hTs = sb("hTs", [128, 16, 256], BF16, 117 * KB)
    hT_b[1] = sb("hT_b1", [128, 16, 512], BF16, 125 * KB)
    hT_b[2] = sb("hT_b2", [128, 16, 512], BF16, 141 * KB)
    xs = [sb("xs%d" % i, [128, D], F32, (157 + 8 * i) * KB) for i in range(6)]

    xs_ctr = [0]

    def load_norm_tile(src_ap, rows):
        slot = xs[xs_ctr[0] % 6]
        k = xs_ctr[0] % 6
        xs_ctr[0] += 1
        dma('sp', slot[:rows, :], src_ap, 'xs%d' % k)
        rstd, nmr = ln_stats(slot[:rows, :], rows, LN_EPS)
        act(slot[:rows, :], slot[:rows, :], AF.Identity, bias=nmr, scale=rstd)
        return slot

    ev_ctr = [0]

    def evac_gb(out_ap, in_ap, c, rows=128):
        g = lnin_cols[:rows, c:c + 1]
        b = lnin_cols[:rows, 16 + c:17 + c]
        if ev_ctr[0] % 2 == 0:
            act(out_ap, in_ap, AF.Identity, bias=b, scale=g)
        else:
            I('dve', 'tensor_scalar', out_ap, in_ap, g, b, ALU.mult, ALU.add)
        ev_ctr[0] += 1

    _skipab = _os.environ.get('SKIPAB', '0') == '1'
    for g in range(4 if (stop >= 0 and not _skipab) else 0):
        slots = [load_norm_tile(T.xall[(4 * g + t) * 128:(4 * g + t + 1) * 128, :], 128) for t in range(4)]
        for cq in range(4 if _os.environ.get('A0CUT', '0') not in ('1',) else 0):
            for t in range(4):
                for c in range(4 * cq, 4 * cq + 4):
                    tp(ps[c % 4 + 4 * (cq % 2)][:, t * 128:(t + 1) * 128], slots[t][:, c * 128:(c + 1) * 128], ident[:])
            for c in range(4 * cq, 4 * cq + 4):
                evac_gb(hT_b[g][:, c, :], ps[c % 4 + 4 * (cq % 2)][:, :], c)
    _cut = _os.environ.get('A0CUT', '0')
    if stop >= 0 and _cut in ('0', '3', '4') and not _skipab:
        slots = [load_norm_tile(T.xall[2048:2176, :], 128), load_norm_tile(T.xhalo, 128)]
        for cq in range(4 if _cut != '4' else 0):
            for t in range(2):
                for c in range(4 * cq, 4 * cq + 4):
                    tp(ps[c % 4 + 4 * (cq % 2)][:, t * 128:(t + 1) * 128], slots[t][:, c * 128:(c + 1) * 128], ident[:])
            for c in range(4 * cq, 4 * cq + 4) if _cut != '3' else ():
                evac_gb(hTs[:, c, :], ps[c % 4 + 4 * (cq % 2)][:, 0:256], c)

    if debug:
        dbg['hT_b1'] = (hT_b[1], [128, 16, 512], BF16)
        dbg['hTs'] = (hTs, [128, 16, 256], BF16)

    wr = [sb("wr%d" % i, [128, 16, 256], BF16, (157 + 8 * i) * KB) for i in range(3)]
    sqt = [sb("sqt%d" % i, [128, 512], BF16, (181 + i) * KB) for i in range(2)]
    tmpA = sb("tmpA", [128, 512], F32, 183 * KB)
    tmpB = sb("tmpB", [128, 512], F32, 185 * KB)

    def wslice(w_ap, c0, n):
        return w_ap[:, c0:c0 + n].rearrange("(kc p) n -> p kc n", p=128)

    if stop >= 1 and not _skipab:
        dma('pool', wr[0][:], wslice(T.w_in, 768, 256), 'wr0')
        dma('pool', wr[1][:], wslice(T.w_in, 1024, 256), 'wr1')
        dmas('pool', [(wr[2][:, :, 0:64], wslice(T.w_in, 1280, 64)),
                      (wr[2][:, :, 64:96], wslice(T.w_in, 1312, 32)),
                      (wr[2][:, :, 96:128], wslice(T.w_in, 1280, 32))], 'wr2')
        blocks = [(hT_b[0], 0, 512), (hT_b[1], 512, 512), (hT_b[2], 1024, 512), (hT_b[3], 1536, 512), (None, 2048, 128)]
        bk = 0
        for (hb, col0, n) in blocks:
            def rhs(kc):
                return hb[:, kc, :] if hb is not None else hTs[:, kc, 0:128]
            pS = ps[2]
            for m in range(4):
                pA = ps[bk % 2]
                bk += 1
                for kc in range(16):
                    mm(pA[:, 0:n], wr[m // 2][:, kc, (m % 2) * 128:(m % 2) * 128 + 128], rhs(kc), start=(kc == 0), stop=(kc == 15))
                act(ckvgT[:, m, col0:col0 + n], pA[:, 0:n], AF.Copy, scale=kvg_col[:, m:m + 1])
                sq = sqt[m % 2]
                act(sq[:, 0:n], pA[:, 0:n], AF.Square)
                mm(pS[:, 0:n], ones_b[:], sq[:, 0:n], start=(m == 0), stop=(m == 3))
            I('dve', 'tensor_scalar', tmpA[:, 0:n], pS[:, 0:n], 1.0 / 512.0, RMS_EPS, ALU.mult, ALU.add)
            act(tmpA[:, 0:n], tmpA[:, 0:n], AF.Sqrt)
            I('dve', 'reciprocal', rstd_kv[:, col0:col0 + n], tmpA[:, 0:n])
            pK1 = ps[3]
            pK2 = ps[4]
            for kc in range(16):
                mm(pK1[0:64, 0:n], wr[2][:, kc, 0:64], rhs(kc), start=(kc == 0), stop=(kc == 15))
            for kc in range(16):
                mm(pK2[0:64, 0:n], wr[2][:, kc, 64:128], rhs(kc), start=(kc == 0), stop=(kc == 15))
            I('dve', 'tensor_tensor', tmpA[0:64, 0:n], pK1[0:64, 0:n], cosT[:, col0:col0 + n], ALU.mult)
            I('dve', 'tensor_tensor', tmpB[0:64, 0:n], pK2[0:64, 0:n], sinT[:, col0:col0 + n], ALU.mult)
            I('dve', 'tensor_tensor', KpT[0:64, 0, col0:col0 + n], tmpA[0:64, 0:n], tmpB[0:64, 0:n], ALU.add)
            I('pool', 'tensor_copy', KpT[0:64, 1, col0:col0 + n], KpT[0:64, 0, col0:col0 + n])
        for i in range(17):
            rows = 128
            pR = ps[5 + i % 2]
            tp(pR[:, 0:128], rstd_kv[:, i * 128:(i + 1) * 128], ident[:])
            I('dve', 'tensor_copy', rstd_col[:rows, i:i + 1], pR[:rows, 0:1])
        for j in range(3):
            dma('pool', wr[j][:], wslice(T.w_in, 256 * j, 256), 'wr%d' % j)
        for tb in range(2):
            hb = hT_b[1 + tb]
            pS = ps[2]
            for m in range(6):
                pA = ps[bk % 2]
                bk += 1
                for kc in range(16):
                    mm(pA[:, :], wr[m // 2][:, kc, (m % 2) * 128:(m % 2) * 128 + 128], hb[:, kc, :], start=(kc == 0), stop=(kc == 15))
                act(cqgT[:, m, tb * 512:(tb + 1) * 512], pA[:, :], AF.Copy, scale=qg_col[:, m:m + 1])
                sq = sqt[m % 2]
                act(sq[:, :], pA[:, :], AF.Square)
                mm(pS[:, :], ones_b[:], sq[:, :], start=(m == 0), stop=(m == 5))
            I('dve', 'tensor_scalar', tmpA[:, :], pS[:, :], 1.0 / 768.0, RMS_EPS, ALU.mult, ALU.add)
            act(tmpA[:, :], tmpA[:, :], AF.Sqrt)
            I('dve', 'reciprocal', rstd_q[:, tb * 512:(tb + 1) * 512], tmpA[:, :])
    if debug and stop >= 1:
        dbg['ckvgT'] = (ckvgT, [128, 4, LKP], BF16)
        dbg['KpT'] = (KpT, [128, 2, LKP], BF16)
        dbg['rstd_kv'] = (rstd_kv, [128, LKP], F32)
        dbg['cqgT'] = (cqgT, [128, 6, NOWN], BF16)

    if stop >= 2 and not _skipab:
        conv_out = sb("conv_out", [128, 8, 512], F32, 85 * KB)
        hc = [sb("hc%d" % i, [128, 544], F32, 101 * KB + i * 2176) for i in range(2)]
        sgt = [sb("sgt%d" % i, [128, 544], F32, 101 * KB + 4352 + i * 2176) for i in range(2)]
        cmean = sb("cmean", [128, 512], F32, 101 * KB + 8704)
        crstd = sb("crstd", [128, 512], F32, 101 * KB + 8704 + 2048)
        ctmp = [sb("ctmp%d" % i, [128, 512], F32, (187 + 2 * i) * KB) for i in range(2)]
        wctr = 0
        for blk in range(2):
            hb = hT_b[1 + blk]
            for c in range(8):
                slot = wr[wctr % 3]
                dmas('pool', [(slot[:, :, 0:128], wslice(T.w_in, 1344 + 128 * c, 128)),
                              (slot[:, :, 128:256], wslice(T.w_in, 2368 + 128 * c, 128))], 'wr%d' % (wctr % 3))
                wctr += 1
                pA, pG, pH = ps[0 + 3 * (c % 2)], ps[1 + 3 * (c % 2)], ps[2 + 3 * (c % 2)]
                for kc in range(16):
                    mm(pA[:, :], slot[:, kc, 0:128], hb[:, kc, :], start=(kc == 0), stop=(kc == 15))
                for kc in range(16):
                    mm(pG[:, :], slot[:, kc, 128:256], hb[:, kc, :], start=(kc == 0), stop=(kc == 15))
                hcols = slice(128 + 32 * blk, 128 + 32 * blk + 32)
                for kc in range(16):
                    mm(ps[6][:, 0:32], slot[:, kc, 0:128], hTs[:, kc, hcols], start=(kc == 0), stop=(kc == 15))
                for kc in range(16):
                    mm(ps[7][:, 0:32], slot[:, kc, 128:256], hTs[:, kc, hcols], start=(kc == 0), stop=(kc == 15))
                h_ = hc[c % 2]
                s_ = sgt[c % 2]
                act(s_[:, 32:544], pG[:, :], AF.Sigmoid)
                act(s_[:, 0:32], ps[7][:, 0:32], AF.Sigmoid)
                I('dve', 'tensor_tensor', h_[:, 32:544], pA[:, :], s_[:, 32:544], ALU.mult)
                I('dve', 'tensor_tensor', h_[:, 0:32], ps[6][:, 0:32], s_[:, 0:32], ALU.mult)
                I('dve', 'tensor_tensor', h_[:, 0:32], h_[:, 0:32], hmask[:, 32 * blk:32 * blk + 32], ALU.mult)
                ce = 'dve'
                co = conv_out[:, c, :]
                I(ce, 'tensor_scalar', co, h_[:, 2:514], dw_cols[:, c, 0:1], dwb_col[:, c:c + 1], ALU.mult, ALU.add)
                for k in range(1, 31):
                    I(ce, 'scalar_tensor_tensor', co, h_[:, 2 + k:514 + k], dw_cols[:, c, k:k + 1], co, ALU.mult, ALU.add)
            pM, pQ = ps[6], ps[7]
            for c in range(8):
                mm(pM[:, :], ones_f[:], conv_out[:, c, :], start=(c == 0), stop=(c == 7))
            for c in range(8):
                t_ = ctmp[c % 2]
                act(t_[:, :], conv_out[:, c, :], AF.Square)
                mm(pQ[:, :], ones_f[:], t_[:, :], start=(c == 0), stop=(c == 7))
            I('dve', 'tensor_scalar', cmean[:, :], pM[:, :], 1.0 / 1024.0, None, ALU.mult)
            I('dve', 'tensor_tensor', crstd[:, :], cmean[:, :], cmean[:, :], ALU.mult)
            I('dve', 'scalar_tensor_tensor', crstd[:, :], pQ[:, :], 1.0 / 1024.0, crstd[:, :], ALU.mult, ALU.subtract)
            I('dve', 'tensor_scalar', crstd[:, :], crstd[:, :], LN_EPS, None, ALU.add)
            act(crstd[:, :], crstd[:, :], AF.Sqrt)
            I('dve', 'reciprocal', crstd[:, :], crstd[:, :])
            for c in range(8):
                ce = 'dve' if c % 2 == 0 else 'pool'
                t_ = ctmp[c % 2]
                I(ce, 'tensor_tensor', t_[:, :], conv_out[:, c, :], cmean[:, :], ALU.subtract)
                I(ce, 'tensor_tensor', t_[:, :], t_[:, :], crstd[:, :], ALU.mult)
                act(convT[:, c, blk * 512:(blk + 1) * 512], t_[:, :], AF.Silu, bias=cln_cols[:, 8 + c:9 + c], scale=cln_cols[:, c:c + 1])
    if debug and stop >= 2:
        dbg['convT'] = (convT, [128, 8, NOWN], BF16)

    if stop >= 3 and not _skipab:
        b1 = Bump(85 * KB, 175 * KB)
        V_all = b1("V_all", [128, 17, 1024], BF16)
        w_uv_sb = b1("w_uv_sb", [128, 4, 1024], BF16)
        w_uk_sb = b1("w_uk_sb", [128, 4, 1024], BF16)
        wq = [b1("wq%d" % i, [128, 6, 256], BF16) for i in range(2)]
        KnT = [b1("KnT%d" % i, [128, LKP], BF16) for i in range(2)]
        QnT = [b1("QnT%d" % i, [128, NOWN], BF16) for i in range(2)]
        QpT = [b1("QpT%d" % i, [128, NOWN], BF16) for i in range(2)]
        PT = [b1("PT%d" % i, [128, 512], BF16) for i in range(4)]
        rs = [b1("rs%d" % i, [128, 512], F32) for i in range(2)]
        tq = [b1("tq%d" % i, [64, 512], F32) for i in range(2)]

        dma('pool', w_uv_sb[:], T.w_uv.rearrange("(kc p) n -> p kc n", p=128), 'wuv')
        dma('pool', w_uk_sb[:], T.w_uk.rearrange("(kc p) n -> p kc n", p=128), 'wuk')
        for i in range(2):
            I('pool', 'memset', QpT[i][64:65, :], 1.0)
        for i in range(17):
            rows = 128
            for nb in range(2):
                pV = ps[(2 * i + nb) % 2]
                for kc in range(4):
                    mm(pV[:rows, :], ckvgT[:, kc, i * 128:i * 128 + rows], w_uv_sb[:, kc, nb * 512:(nb + 1) * 512], start=(kc == 0), stop=(kc == 3))
                act(V_all[:rows, i, nb * 512:(nb + 1) * 512], pV[:rows, :], AF.Copy, scale=rstd_col[:rows, i:i + 1])
        ptc = 0
        sbank = 0
        for h in range(8):
            hb_ = h % 2
            wqh = wq[hb_]
            dmas('pool', [(wqh[:, :, 0:192], T.w_uq[:, h * 192:(h + 1) * 192].rearrange("(kc p) n -> p kc n", p=128)),
                          (wqh[:, :, 192:224], T.w_uq[:, h * 192 + 160:h * 192 + 192].rearrange("(kc p) n -> p kc n", p=128)),
                          (wqh[:, :, 224:256], T.w_uq[:, h * 192 + 128:h * 192 + 160].rearrange("(kc p) n -> p kc n", p=128))],
                 'wq%d' % hb_)
            kn, qn, qp = KnT[hb_], QnT[hb_], QpT[hb_]
            for tb in range(2):
                cols = slice(tb * 512, (tb + 1) * 512)
                pcols = slice(512 + tb * 512, 1024 + tb * 512)
                pA = ps[0]
                for kc in range(6):
                    mm(pA[:, :], wqh[:, kc, 0:128], cqgT[:, kc, cols], start=(kc == 0), stop=(kc == 5))
                I('dve', 'tensor_tensor', qn[:, cols], pA[:, :], rstd_q[:, cols], ALU.mult)
                pX, pXs = ps[1], ps[2]
                for kc in range(6):
                    mm(pX[0:64, :], wqh[:, kc, 128:192], cqgT[:, kc, cols], start=(kc == 0), stop=(kc == 5))
                for kc in range(6):
                    mm(pXs[0:64, :], wqh[:, kc, 192:256], cqgT[:, kc, cols], start=(kc == 0), stop=(kc == 5))
                I('dve', 'tensor_tensor', tq[0][:, :], pX[0:64, :], cosT[:, pcols], ALU.mult)
                I('dve', 'tensor_tensor', tq[1][:, :], pXs[0:64, :], sinT[:, pcols], ALU.mult)
                I('dve', 'tensor_tensor', tq[0][:, :], tq[0][:, :], tq[1][:, :], ALU.add)
                I('dve', 'tensor_tensor', qp[0:64, cols], tq[0][:, :], rstd_q[0:64, cols], ALU.mult)
            for (col0, n) in ((0, 512), (512, 512), (1024, 512), (1536, 512), (2048, 128)):
                pA = ps[3]
                for kc in range(4):
                    mm(pA[:, 0:n], w_uk_sb[:, kc, h * 128:(h + 1) * 128], ckvgT[:, kc, col0:col0 + n], start=(kc == 0), stop=(kc == 3))
                I('dve', 'tensor_tensor', kn[:, col0:col0 + n], pA[:, 0:n], rstd_kv[:, col0:col0 + n], ALU.mult)
            for s in range(2):
                q0 = s * 512
                if s == 0:
                    full = [0, 1, 2, 3]
                    diag = [4, 5, 6, 7]
                else:
                    full = [0, 1, 2, 3, 4, 5, 6, 7, 12, 13, 14, 15]
                    diag = [8, 9, 10, 11]
                tiles = [(kt, 128, 0) for kt in full] + [(16, 128, 0)] + [(kt, 128, r) for r, kt in enumerate(diag)]
                pO = ps[4 + (2 * h + s) % 2]
                pZ = ps[6 + (2 * h + s) % 2]
                for ti, (kt, rows, r) in enumerate(tiles):
                    qoff = 128 * r
                    n = 512 - qoff
                    kc0 = kt * 128
                    pS = ps[sbank % 3]
                    sbank += 1
                    mm(pS[:rows, 0:n], kn[:, kc0:kc0 + rows], qn[:, q0 + qoff:q0 + 512], start=True, stop=False)
                    mm(pS[:rows, 0:n], KpT[0:65, s, kc0:kc0 + rows], qp[0:65, q0 + qoff:q0 + 512], start=False, stop=True)
                    pt = PT[ptc % 4]
                    ptc += 1
                    act(pt[:rows, 0:n], pS[:rows, 0:n], AF.Exp, scale=QSCALE)
                    if ti > len(full):
                        I('pool', 'tensor_tensor', pt[:, 0:128], pt[:, 0:128], tri_b[:], ALU.mult)
                    first = (ti == 0)
                    last = (ti == len(tiles) - 1)
                    mm(pO[:, qoff:512], V_all[:rows, kt, h * 128:(h + 1) * 128], pt[:rows, 0:n], start=first, stop=last)
                    mm(pZ[:, qoff:512], ones_b[:rows, :], pt[:rows, 0:n], start=first, stop=last)
                r_ = rs[(2 * h + s) % 2]
                I('dve', 'reciprocal', r_[:, :], pZ[:, :])
                I('dve', 'tensor_tensor', attnT[:, h, q0:q0 + 512], pO[:, :], r_[:, :], ALU.mult)
    if debug and stop >= 3:
        dbg['attnT'] = (attnT, [128, 8, NOWN], BF16)

    acc = sb("acc", [128, 8, D], F32, 18 * KB)
    h1b = sb("h1b", [128, 8, D], BF16, 82 * KB)
    if stop >= 4 and not _skipab:
        wo = [sb("wo%d" % i, [128, 16, 256], BF16, (114 + 8 * i) * KB) for i in range(3)]
        xs2 = [sb("xs2_%d" % i, [128, D], F32, (138 + 8 * i) * KB) for i in range(2)]
        lng = sb("lng", [128, D], F32, 154 * KB)
        lnb = sb("lnb", [128, D], F32, 162 * KB)
        h1T = sb("h1T", [128, 16, 128], F32, 138 * KB)
        lgt = sb("lgt", [128, 4, NE], F32, 170 * KB)
        m8 = sb("m8", [128, 16], F32, 170 * KB + 512)
        dmas('sp', [(lng[:], T.lnin_bc_d[0]), (lnb[:], T.lnin_bc_d[1])], 'lnbc')
        for i in range(8):
            slot = xs2[i % 2]
            dma('sp', slot[:], T.xall[512 + i * 128:512 + (i + 1) * 128, :], 'xs2_%d' % (i % 2))
            rstd, nmr = ln_stats(slot[:], 128, LN_EPS)
            act(slot[:], slot[:], AF.Identity, bias=nmr, scale=rstd)
            I('dve', 'tensor_tensor', acc[:, i, :], slot[:], lng[:], ALU.mult)
            I('pool', 'tensor_tensor', acc[:, i, :], acc[:, i, :], lnb[:], ALU.add)
        for nb in range(8):
            slot = wo[nb % 3]
            dma('pool', slot[:], wslice(T.w_out, nb * 256, 256), 'wo%d' % (nb % 3))
            for i in range(8):
                pA = ps[(nb * 8 + i) % 4]
                for kc in range(16):
                    lhs = attnT[:, kc, i * 128:(i + 1) * 128] if kc < 8 else convT[:, kc - 8, i * 128:(i + 1) * 128]
                    mm(pA[:, 0:256], lhs, slot[:, kc, :], start=(kc == 0), stop=(kc == 15))
                I('dve', 'scalar_tensor_tensor', acc[:, i, nb * 256:(nb + 1) * 256], acc[:, i, nb * 256:(nb + 1) * 256], ALPHA, pA[:, 0:256], ALU.mult, ALU.add)
        dmas('sp', [(lng[:], T.ln1_bc_d[0]), (lnb[:], T.ln1_bc_d[1])], 'lnbc')
        _b2cut = _os.environ.get('B2CUT', 'Z')
        for i in range(8 if _b2cut != 'A' else 0):
            a_ = acc[:, i, :]
            rstd, nmr = ln_stats(a_, 128, LN_EPS)
            act(a_, a_, AF.Identity, bias=nmr, scale=rstd)
            I('dve', 'tensor_tensor', a_, a_, lng[:], ALU.mult)
            I('pool', 'tensor_tensor', a_, a_, lnb[:], ALU.add)
            I('pool', 'tensor_copy', h1b[:, i, :], a_)
            if _b2cut == 'B':
                continue
            for cq in range(4):
                pT = ps[cq % 2]
                for c in range(4 * cq, 4 * cq + 4):
                    tp(pT[:, (c % 4) * 128:(c % 4) * 128 + 128], acc[:, i, c * 128:(c + 1) * 128], ident[:])
                act(h1T[:, 4 * cq:4 * cq + 4, :], pT[:, :].rearrange("p (c t) -> p c t", c=4), AF.Copy)
            pL = ps[2]
            for c in range(16):
                mm(pL[:, 0:NE], h1T[:, c, :], wr_sb[:, c, :], start=(c == 0), stop=(c == 15))
            lg = lgt[:, 0, :]
            ex = lgt[:, 1, :]
            mk = lgt[:, 2, :]
            tm = lgt[:, 3, :]
            I('dve', 'tensor_tensor', lg, pL[:, 0:NE], brouter[:], ALU.add)
            I('dve', 'max', m8[:, 0:8], lg)
            I('dve', 'tensor_scalar', mk, lg, m8[:, 3:4], None, ALU.is_ge)
            I('dve', 'tensor_scalar', m8[:, 8:9], m8[:, 0:1], -1.0, None, ALU.mult)
            act(ex, lg, AF.Exp, bias=m8[:, 8:9], scale=1.0)
            I('dve', 'tensor_tensor', ex, ex, mk, ALU.mult)
            I('dve', 'tensor_reduce', m8[:, 9:10], ex, mybir.AxisListType.X, ALU.add)
            I('dve', 'reciprocal', m8[:, 10:11], m8[:, 9:10])
            I('dve', 'tensor_scalar', Gt[:, i, :], ex, m8[:, 10:11], None, ALU.mult)
            I('dve', 'tensor_copy', maskb[:, i, :], mk)
            I('dve', 'tensor_copy', Ghl[:, i, :, 0], Gt[:, i, :])
            I('dve', 'tensor_tensor', tm, Gt[:, i, :], Ghl[:, i, :, 0], ALU.subtract)
            I('dve', 'tensor_copy', Ghl[:, i, :, 1], tm)
            if _b2cut == 'C':
                continue
            pP = ps[3]
            for ip in range(i):
                mm(pP[:, 0:NE], ones_b[:], maskb[:, ip, :], start=(ip == 0), stop=False)
            mm(pP[:, 0:NE], ustr_b[:], maskb[:, i, :], start=(i == 0), stop=True)
            I('dve', 'scalar_tensor_tensor', posm[:, i, :], pP[:, 0:NE], 1.0, mk, ALU.add, ALU.mult)
            I('dve', 'tensor_scalar', posm[:, i, :], posm[:, i, :], -1.0, None, ALU.add)
            if _b2cut == 'D':
                continue
            I('dve', 'tensor_scalar', acc[:, i, :], acc[:, i, :], ALPHA, None, ALU.mult)
    if debug and stop >= 4:
        dbg['h1b'] = (h1b, [128, 8, D], BF16)
        dbg['Gt'] = (Gt, [128, 8, NE], F32)
        dbg['posm'] = (posm, [128, 8, NE], F32)
        dbg['acc0'] = (acc, [128, 8, D], F32)

    if stop >= 5:
        wring = [sb("wring%d" % i, [128, 16, 512], BF16, (114 + 16 * i) * KB) for i in range(3)]
        xgT = sb("xgT", [128, 16, CAP], BF16, 162 * KB)
        actT = sb("actT", [128, 16, CAP], BF16, 170 * KB)
        y_e = sb("y_e", [128, 2, D], BF16, 178 * KB)
        S_ = sb("S_", [128, 8, CAP], BF16, 186 * KB)
        ST = [sb("ST%d" % i, [128, 2, NOWN], BF16, (190 + 4 * i) * KB) for i in range(2)]
        tg = [sb("tg%d" % i, [128, CAP], F32, 198 * KB + i * 1024) for i in range(2)]
        ts_ = [sb("ts%d" % i, [128, CAP], F32, 200 * KB + i * 1024) for i in range(2)]
        tu = [sb("tu%d" % i, [128, CAP], F32, 202 * KB + i * 1024) for i in range(2)]
        b2row = [sb("b2row%d" % i, [1, 512], BF16, 204 * KB + i * 1024) for i in range(2)]
        ne_run = NE if stop >= 6 else 2
        wctr = [0]

        def wload(pairs_fn):
            k = wctr[0] % 3
            wctr[0] += 1
            slot = wring[k]
            dmas('pool', pairs_fn(slot), 'wring%d' % k)
            return slot

        def build_S(e):
            for i in range(8):
                I('dve', 'tensor_scalar', S_[:, i, :], iota_j[:], posm[:, i, e:e + 1], None, ALU.is_equal)

        def gather(e):
            for dp in range(8):
                pX = ps[6 + dp % 2]
                for half in range(2):
                    dc = 2 * dp + half
                    for i in range(8):
                        mm(pX[:, half * CAP:(half + 1) * CAP], h1b[:, i, dc * 128:(dc + 1) * 128], S_[:, i, :], start=(i == 0), stop=(i == 7))
                if dp % 2 == 0:
                    act(xgT[:, 2 * dp:2 * dp + 2, :], pX[:, :].rearrange("p (c t) -> p c t", c=2), AF.Copy)
                else:
                    I('dve', 'tensor_copy', xgT[:, 2 * dp:2 * dp + 2, :], pX[:, :].rearrange("p (c t) -> p c t", c=2))
            pg = ps[2]
            for jc in range(2):
                for i in range(8):
                    mm(pg[:, 2 * jc:2 * jc + 2], S_[:, i, jc * 128:(jc + 1) * 128], Ghl[:, i, e, :], start=(i == 0), stop=(i == 7))
            gg = gate_g[:, e % 2, :]
            I('dve', 'tensor_copy', smallf[:, 200:204], pg[:, 0:4])
            I('dve', 'tensor_tensor', gg, smallf[:, 200:204:2], smallf[:, 201:204:2], ALU.add)
            st = ST[e % 2]
            for jc in range(2):
                pT = ps[4 + jc][:].bitcast(BF16)
                for i in range(8):
                    tp(pT[:, i * 128:(i + 1) * 128], S_[:, i, jc * 128:(jc + 1) * 128], ident_b[:])
                act(st[:, jc, :], pT[:, 0:NOWN], AF.Copy)

        def mm1(e):
            for wb in range(8):
                slot = wload(lambda s_: [(s_[:, :, 0:256], T.w1[e][:, wb * 256:(wb + 1) * 256].rearrange("(kc p) n -> p kc n", p=128)),
                                         (s_[:, :, 256:512], T.w1[e][:, DFF + wb * 256:DFF + (wb + 1) * 256].rearrange("(kc p) n -> p kc n", p=128))])
                for cc in range(2):
                    c = 2 * wb + cc
                    pZ = ps[c % 2]
                    for kc in range(16):
                        mm(pZ[:, 0:CAP], slot[:, kc, cc * 128:(cc + 1) * 128], xgT[:, kc, :], start=(kc == 0), stop=(kc == 15))
                    for kc in range(16):
                        mm(pZ[:, CAP:2 * CAP], slot[:, kc, 256 + cc * 128:256 + (cc + 1) * 128], xgT[:, kc, :], start=(kc == 0), stop=(kc == 15))
                    g_, s2, u_ = tg[c % 2], ts_[c % 2], tu[c % 2]
                    _m1 = _os.environ.get('MM1CUT', 'z')
                    if _m1 == 'a':
                        continue
                    I('dve', 'tensor_scalar', g_[:, :], pZ[:, 0:CAP], b1_cols[:, e, c:c + 1], 7.0, ALU.add, ALU.min, xr=[pZ[:, :]])
                    act(s2[:, :], g_[:, :], AF.Sigmoid, scale=1.702)
                    I('dve', 'tensor_scalar', u_[:, :], pZ[:, CAP:2 * CAP], b1_cols[:, e, 16 + c:17 + c], 7.0, ALU.add, ALU.min)
                    if _m1 == 'b':
                        continue
                    I('pool', 'tensor_scalar', u_[:, :], u_[:, :], -7.0, 1.0, ALU.max, ALU.add)
                    I('pool', 'tensor_tensor', g_[:, :], g_[:, :], s2[:, :], ALU.mult)
                    I('pool', 'tensor_tensor', actT[:, c, :], g_[:, :], u_[:, :], ALU.mult)

        def mm2(e):
            for nb in range(4):
                slot = wload(lambda s_: [(s_[:], T.w2[e][:, nb * 512:(nb + 1) * 512].rearrange("(kc p) n -> p kc n", p=128))])
                br = b2row[nb % 2]
                dma('pool', br[0:1, :], T.b2_d[e:e + 1, nb * 512:(nb + 1) * 512], 'b2r%d' % (nb % 2))
                for jt in range(2):
                    pY = ps[2 + jt]
                    for kc in range(16):
                        mm(pY[:, :], actT[:, kc, jt * 128:(jt + 1) * 128], slot[:, kc, :], start=(kc == 0), stop=False)
                    mm(pY[:, :], ones_b[0:1, :], br[0:1, :], start=False, stop=True)
                    act(y_e[:, jt, nb * 512:(nb + 1) * 512], pY[:, :], AF.Copy, scale=gate_g[:, e % 2, jt:jt + 1])

        def scatter(e):
            st = ST[e % 2]
            k = 0
            for i in range(8):
                for nb in range(4):
                    pC = ps[4 + k % 2]
                    k += 1
                    mm(pC[:, :], st[:, 0, i * 128:(i + 1) * 128], y_e[:, 0, nb * 512:(nb + 1) * 512], start=True, stop=False)
                    mm(pC[:, :], st[:, 1, i * 128:(i + 1) * 128], y_e[:, 1, nb * 512:(nb + 1) * 512], start=False, stop=True)
                    I('dve', 'tensor_tensor', acc[:, i, nb * 512:(nb + 1) * 512], acc[:, i, nb * 512:(nb + 1) * 512], pC[:, :], ALU.add)

        _mc = _os.environ.get('MOECUT', '9')
        build_S(0)
        gather(0)
        for e in range(ne_run if _mc == '9' else 1):
            if _mc >= '2':
                mm1(e)
            if e + 1 < ne_run and _mc == '9':
                build_S(e + 1)
                gather(e + 1)
            if _mc >= '3':
                mm2(e)
            if _mc >= '4':
                scatter(e)

    if stop >= 4:
        lng2 = sb("lng2", [128, D], F32, 114 * KB)
        lnb2 = sb("lnb2", [128, D], F32, 122 * KB)
        dmas('sp', [(lng2[:], T.ln2_bc_d[0]), (lnb2[:], T.ln2_bc_d[1])], 'lnbc2')
        outs = []
        for i in range(8):
            a_ = acc[:, i, :]
            rstd, nmr = ln_stats(a_, 128, LN_EPS)
            act(a_, a_, AF.Identity, bias=nmr, scale=rstd)
            I('dve', 'tensor_tensor', a_, a_, lng2[:], ALU.mult)
            I('pool', 'tensor_tensor', a_, a_, lnb2[:], ALU.add)
            outs.append(dma('sp', out_d[i * 128:(i + 1) * 128, :], a_, 'out%d' % (i % 4)))
    else:
        outs = []
        outs.append(dma('sp', out_d[0:128, 0:256], smallf[:, 0:256], 'out0'))

    for name, (h, shape, dt) in dbg.items():
        dd = nc.dram_tensor("dbg_" + name, list(shape), dt, kind="ExternalOutput").ap()
        outs.append(dma('sp', dd, h[:], 'dbg'))

    fin = P.op('sp', lambda e: e.nop(), r=(), w=())
    P.ops[fin]['deps'] = set(outs)
    P.emit()
    nc._used_inputs = list(used_inputs)
    return nc


def _perm(s):
    return [0, 1, 2, 3] if s == 1 else [1, 0, 3, 2]


def _rope_tables():
    inv_freq = (1.0 / (10000.0 ** (np.arange(0, 64, 2, dtype=np.float32) / np.float32(64)))).astype(np.float32)
    pos = np.arange(LK, dtype=np.float32)
    freqs = (pos[:, None] * inv_freq[None, :]).astype(np.float32)
    emb = np.concatenate([freqs, freqs], axis=-1)
    cos = np.cos(emb).astype(np.float32)
    sin = np.sin(emb).astype(np.float32)
    sign = np.concatenate([-np.ones(32, np.float32), np.ones(32, np.float32)])
    return cos, sin * sign[None, :]


def make_in_maps(inputs, ne_alloc=NE):
    f = lambda a: np.ascontiguousarray(np.asarray(a, dtype=np.float32))
    x = f(inputs["x"])
    meta = f(inputs["meta_tokens"])
    cos, sinS = _rope_tables()
    shared = {}
    shared["ident"] = np.eye(128, dtype=np.float32)
    shared["tri"] = np.triu(np.ones((128, 128), np.float32))
    shared["ustrict"] = np.triu(np.ones((128, 128), np.float32), 1)
    shared["iota"] = np.ascontiguousarray(np.broadcast_to(np.arange(CAP, dtype=np.float32)[None, :], (128, CAP)))
    shared["w_in"] = f(inputs["w_in"][0])
    shared["w_uq"] = f(inputs["w_uq"][0])
    shared["w_uk"] = f(inputs["w_uk"][0])
    shared["w_uv"] = f(inputs["w_uv"][0])
    shared["w_out"] = f(inputs["w_out"][0])
    shared["w_router"] = f(inputs["w_router"][0])
    shared["w1"] = f(inputs["w_mlp1"][0][:ne_alloc])
    shared["w2"] = f(inputs["w_mlp2"][0][:ne_alloc])
    cols = lambda v, n: np.ascontiguousarray(f(v).reshape(n, 128).T)
    bc = lambda g, b: np.ascontiguousarray(np.stack([np.broadcast_to(f(g)[None, :], (128, D)), np.broadcast_to(f(b)[None, :], (128, D))]))
    shared["lnin_cols"] = np.ascontiguousarray(np.concatenate([cols(inputs["ln_in_g"], 16), cols(inputs["ln_in_b"], 16)], axis=1))
    shared["lnin_bc"] = bc(inputs["ln_in_g"], inputs["ln_in_b"])
    shared["ln1_bc"] = bc(inputs["ln1_g"][0], inputs["ln1_b"][0])
    shared["ln2_bc"] = bc(inputs["ln2_g"][0], inputs["ln2_b"][0])
    shared["qg_col"] = cols(inputs["q_norm_g"][0], 6)
    shared["kvg_col"] = cols(inputs["kv_norm_g"][0], 4)
    dw = f(inputs["conv_dw_w"][0])
    shared["dw_cols"] = np.ascontiguousarray(dw.reshape(31, 8, 128).transpose(2, 1, 0))
    shared["dwb_col"] = cols(inputs["conv_dw_b"][0], 8)
    shared["cln_cols"] = np.ascontiguousarray(np.concatenate([cols(inputs["conv_ln_g"][0], 8), cols(inputs["conv_ln_b"][0], 8)], axis=1))
    shared["b1_cols"] = np.ascontiguousarray(f(inputs["b_mlp1"][0]).reshape(NE, 32, 128).transpose(2, 0, 1))
    shared["b2"] = f(inputs["b_mlp2"][0])
    shared["brouter_bc"] = np.ascontiguousarray(np.broadcast_to(f(inputs["b_router"][0])[None, :], (128, NE)))
    in_maps = []
    for core in range(8):
        b, s = core // 2, core % 2
        perm = _perm(s)
        xb = x[b]
        rows = np.concatenate([xb[512 * tb:512 * (tb + 1)] for tb in perm] + [meta, np.zeros((112, D), np.float32)], axis=0)
        seqpos = np.concatenate([16 + 512 * tb + np.arange(512) for tb in perm] + [np.arange(16)])
        m = dict(shared)
        m["xall"] = np.ascontiguousarray(rows)
        pad = np.zeros((64, LKP - LK), np.float32)
        m["cosT"] = np.ascontiguousarray(np.concatenate([cos[seqpos].T, pad], axis=1))
        m["sinT"] = np.ascontiguousarray(np.concatenate([sinS[seqpos].T, pad], axis=1))
        kbias = np.zeros((2, LKP), np.float32)
        kbias[:, LK:] = NEG
        for slot, qpb in ((0, 1), (1, 2)):
            for pb in range(4):
                if pb != qpb and not (perm[pb] < perm[qpb]):
                    kbias[slot, 512 * pb:512 * (pb + 1)] = NEG
        m["kbias"] = kbias
        halo = np.zeros((128, D), np.float32)
        hm = np.zeros((64,), np.float32)
        seq = np.concatenate([meta, xb], axis=0)
        for blk, qpb in enumerate((1, 2)):
            t0 = 16 + 512 * perm[qpb]
            for j in range(32):
                sp_ = t0 - 32 + j
                if sp_ >= 0:
                    halo[32 * blk + j] = seq[sp_]
                    hm[32 * blk + j] = 1.0
        m["xhalo"] = halo
        m["halomask"] = np.ascontiguousarray(np.broadcast_to(hm[None, :], (128, 64)))
        in_maps.append(m)
    return in_maps


def assemble(results):
    out = np.zeros((4, SEQ, D), np.float32)
    for core in range(8):
        b, s = core // 2, core % 2
        perm = _perm(s)
        o = np.asarray(results[core]["out"], dtype=np.float32)
        out[b, 512 * perm[1]:512 * (perm[1] + 1)] = o[0:512]
        out[b, 512 * perm[2]:512 * (perm[2] + 1)] = o[512:1024]
    return out


def kernel(**inputs):
    nc = build_nc()
    in_maps = make_in_maps(inputs)
    in_maps = [{k: m[k] for k in nc._used_inputs} for m in in_maps]
    res = run_bass_kernel_spmd(nc, in_maps, core_ids=list(range(8)))
    return assemble(res.results)
```

```python
import math
from contextlib import ExitStack
import numpy as np
import concourse.bass as bass
import concourse.mybir as mybir
from concourse.bass_utils import run_bass_kernel_spmd

F32 = mybir.dt.float32
BF16 = mybir.dt.bfloat16
AF = mybir.ActivationFunctionType
ALU = mybir.AluOpType

D = 2048
SEQ = 2048
NMETA = 16
LK = SEQ + NMETA
LKP = SEQ + 128
NOWN = 1024
NE = 32
CAP = 256
DFF = 2048
ALPHA = 2.0 ** 0.25
LN_EPS = 1e-5
RMS_EPS = 1e-6
QSCALE = 1.0 / math.sqrt(192.0)
NEG = -30000.0
CELL = 32
SB_LIMIT = 212000
SB_BASE = 16640

ENGS = ['pe', 'act', 'dve', 'pool', 'sp']


def _is_ap(x):
    return hasattr(x, 'ap') and hasattr(x, 'tensor') and hasattr(x, 'offset')


class Prog:
    def __init__(self, nc):
        self.nc = nc
        self.ops = []
        self.vbase = {}
        ncell = (SB_LIMIT + 8 * 2048 + 4096) // CELL + 8
        self.last_w = np.full(ncell, -1, np.int64)
        self.last_r = {e: np.full(ncell, -1, np.int64) for e in ENGS}
        self.cache = {}
        self.dma_keys = {}
        self.last_dma = {}

    def reg(self, handle, vbase):
        self.vbase[handle.name] = vbase

    def cells(self, ap):
        sp = str(ap.space)
        if 'DRAM' in sp.upper():
            return None
        if 'PSUM' in sp.upper():
            key = ('psum', ap.tensor.name)
            c = self.cache.get(key)
            if c is None:
                base = self.vbase[ap.tensor.name]
                c = np.arange(base // CELL, (base + 2048) // CELL)
                self.cache[key] = c
            return c
        key = (ap.tensor.name, int(ap.offset), tuple(tuple(x) for x in ap.ap), str(ap.dtype))
        c = self.cache.get(key)
        if c is not None:
            return c
        es = mybir.dt.size(ap.dtype)
        dims = [tuple(x) for x in ap.ap]
        pstep = dims[0][0]
        off = int(ap.offset) % pstep if pstep > 0 else int(ap.offset)
        base = self.vbase[ap.tensor.name]
        idx = np.array([off], dtype=np.int64)
        free = dims[1:]
        if len(free) == 0:
            free = [(1, 1)]
        for step, cnt in free[:-1]:
            idx = (idx[:, None] + (np.arange(cnt, dtype=np.int64) * step)[None, :]).ravel()
        step, cnt = free[-1]
        if step == 1 or cnt == 1:
            lo = base + idx * es
            hi = base + (idx + cnt) * es - 1
        else:
            idx = (idx[:, None] + (np.arange(cnt, dtype=np.int64) * step)[None, :]).ravel()
            lo = base + idx * es
            hi = lo + es - 1
        lo = lo // CELL
        hi = hi // CELL
        n = int((hi - lo).max()) + 1
        c = np.unique((lo[:, None] + np.arange(n)[None, :]).clip(max=hi[:, None]))
        self.cache[key] = c
        return c

    def op(self, eng, fn, r=(), w=(), dma=None, ndma=0):
        i = len(self.ops)
        deps = set()
        rcells = [c for c in (self.cells(a) for a in r) if c is not None]
        wcells = [c for c in (self.cells(a) for a in w) if c is not None]
        raw = set()
        for c in rcells:
            u = np.unique(self.last_w[c])
            raw.update(int(x) for x in u if x >= 0)
        deps |= raw
        for c in wcells:
            u = np.unique(self.last_w[c])
            deps.update(int(x) for x in u if x >= 0)
            for e in ENGS:
                u = np.unique(self.last_r[e][c])
                deps.update(int(x) for x in u if x >= 0)
        keep = set()
        for d in deps:
            od = self.ops[d]
            if od['eng'] == eng and od['dma'] is None and dma is None:
                if eng == 'pe':
                    continue
                if d not in raw:
                    continue
            keep.add(d)
        if dma is not None and dma in self.last_dma:
            keep.add(self.last_dma[dma])
        for c in rcells:
            self.last_r[eng][c] = i
        for c in wcells:
            self.last_w[c] = i
            for e in ENGS:
                self.last_r[e][c] = -1
        self.ops.append(dict(eng=eng, fn=fn, deps=keep, dma=dma, ndma=ndma, signal=False))
        if dma is not None:
            self.last_dma[dma] = i
        return i

    def selfcheck(self, by_eng):
        ops = self.ops
        sem = {}
        pc = {e: 0 for e in ENGS}
        progress = True
        while progress:
            progress = False
            for e in ENGS:
                lst = by_eng[e]
                while pc[e] < len(lst):
                    o = lst[pc[e]]
                    ok = True
                    for d in o['deps']:
                        if 'sig' not in ops[d]:
                            raise RuntimeError("dep on unsignaled op %d" % d)
                        sn, val = ops[d]['sig']
                        if sem.get(sn, 0) < val:
                            ok = False
                            break
                    if not ok:
                        break
                    if o['dma'] is not None:
                        sn = 'd:' + o['dma']
                        sem[sn] = sem.get(sn, 0) + 16 * o['ndma']
                    elif o['signal']:
                        sn = 'e:' + o['eng']
                        sem[sn] = sem.get(sn, 0) + 1
                    pc[e] += 1
                    progress = True
        for e in ENGS:
            if pc[e] < len(by_eng[e]):
                o = by_eng[e][pc[e]]
                raise RuntimeError("deadlock: engine %s stuck at op %d/%d deps=%s" % (e, pc[e], len(by_eng[e]), [(d, ops[d]['eng'], ops[d].get('sig')) for d in o['deps']]))

    def emit(self):
        nc = self.nc
        ops = self.ops
        for o in ops:
            for d in o['deps']:
                ops[d]['signal'] = True
        cnt = {e: 0 for e in ENGS}
        dcnt = {}
        for o in ops:
            if o['dma'] is not None:
                k = o['dma']
                dcnt[k] = dcnt.get(k, 0) + 16 * o['ndma']
                o['sig'] = ('d:' + k, dcnt[k])
            elif o['signal']:
                cnt[o['eng']] += 1
                o['sig'] = ('e:' + o['eng'], cnt[o['eng']])
        semnames = ['e:' + e for e in ENGS] + ['d:' + k for k in dcnt]
        print('PROG tickets', cnt, 'dma', {k: v for k, v in dcnt.items() if v > 1000}, 'nops', len(ops), flush=True)
        by_eng = {e: [o for o in ops if o['eng'] == e] for e in ENGS}
        self.selfcheck(by_eng)
        with ExitStack() as st:
            sems = {}
            for j, n in enumerate(semnames):
                sems[n] = st.enter_context(nc.semaphore("s%d" % j))
            block = st.enter_context(nc.Block())

            def run(e, lst):
                waited = {}
                for o in lst:
                    need = {}
                    for d in o['deps']:
                        sn, val = ops[d]['sig']
                        if need.get(sn, 0) < val:
                            need[sn] = val
                    for sn, val in need.items():
                        if waited.get(sn, 0) < val:
                            e.wait_ge(sems[sn], val)
                            waited[sn] = val
                    if o['dma'] is not None:
                        o['fn'](e, sems['d:' + o['dma']])
                    else:
                        ins = o['fn'](e)
                        if o['signal']:
                            ins.then_inc(sems['e:' + o['eng']], 1)

            @block.tensor
            def _(e):
                run(e, by_eng['pe'])

            @block.scalar
            def _(e):
                run(e, by_eng['act'])

            @block.vector
            def _(e):
                run(e, by_eng['dve'])

            @block.gpsimd
            def _(e):
                run(e, by_eng['pool'])

            @block.sync
            def _(e):
                run(e, by_eng['sp'])


def build_nc(stop=99, debug=False, ne_alloc=NE):
    nc = bass.Bass("TRN2", target_bir_lowering=False)
    P = Prog(nc)

    def din(name, shape, dt=F32):
        return nc.dram_tensor(name, list(shape), dt, kind="ExternalInput").ap()

    _specs = {
        "xall": ("xall", [2048 + 128, D]),
        "xhalo": ("xhalo", [128, D]),
        "halomask": ("halomask", [128, 64]),
        "cosT_d": ("cosT", [64, LKP]),
        "sinT_d": ("sinT", [64, LKP]),
        "kbias_d": ("kbias", [2, LKP]),
        "ident_d": ("ident", [128, 128]),
        "tri_d": ("tri", [128, 128]),
        "ustr_d": ("ustrict", [128, 128]),
        "iota_d": ("iota", [128, CAP]),
        "w_in": ("w_in", [D, 3392]),
        "w_uq": ("w_uq", [768, 1536]),
        "w_uk": ("w_uk", [512, 1024]),
        "w_uv": ("w_uv", [512, 1024]),
        "w_out": ("w_out", [D, D]),
        "w_router": ("w_router", [D, NE]),
        "w1": ("w1", [ne_alloc, D, 2 * DFF]),
        "w2": ("w2", [ne_alloc, DFF, D]),
        "lnin_cols_d": ("lnin_cols", [128, 32]),
        "lnin_bc_d": ("lnin_bc", [2, 128, D]),
        "ln1_bc_d": ("ln1_bc", [2, 128, D]),
        "ln2_bc_d": ("ln2_bc", [2, 128, D]),
        "qg_col_d": ("qg_col", [128, 6]),
        "kvg_col_d": ("kvg_col", [128, 4]),
        "dw_cols_d": ("dw_cols", [128, 8, 31]),
        "dwb_col_d": ("dwb_col", [128, 8]),
        "cln_cols_d": ("cln_cols", [128, 16]),
        "b1_cols_d": ("b1_cols", [128, NE, 32]),
        "b2_d": ("b2", [NE, D]),
        "brouter_d": ("brouter_bc", [128, NE]),
    }
    used_inputs = []

    class _T:
        def __init__(self):
            self.c = {}

        def __getattr__(self, k):
            c = self.__dict__['c']
            if k not in c:
                n, shp = _specs[k]
                c[k] = din(n, shp)
                used_inputs.append(n)
            return c[k]

    T = _T()
    out_d = nc.dram_tensor("out", [NOWN, D], F32, kind="ExternalOutput").ap()
    dbg = {}

    def sb(name, shape, dt, at):
        nbytes = int(np.prod(shape[1:])) * mybir.dt.size(dt)
        assert at % 32 == 0, (name, at)
        assert at + nbytes <= SB_LIMIT, (name, at, nbytes)
        h = nc.alloc_sbuf_tensor_at(name, list(shape), dt, offset=at + SB_BASE)
        P.reg(h, at)
        return h

    class Bump:
        def __init__(self, lo, hi):
            self.p = lo
            self.hi = hi

        def __call__(self, name, shape, dt):
            nbytes = int(np.prod(shape[1:])) * mybir.dt.size(dt)
            nbytes = (nbytes + 31) // 32 * 32
            at = self.p
            assert at + nbytes <= self.hi, (name, at, nbytes, self.hi)
            self.p += nbytes
            return sb(name, shape, dt, at)

    KB = 1024
    ps = []
    for i in range(8):
        h = nc.alloc_psum_tensor("ps%d" % i, [128, 512], F32)
        P.reg(h, SB_LIMIT + 2048 * i)
        ps.append(h)

    def I(eng, name, *args, **kw):
        w = [args[0]]
        xr = kw.pop('xr', [])
        r = [a for a in args[1:] if _is_ap(a)] + [v for k, v in kw.items() if _is_ap(v) and k != 'accum_out'] + list(xr)
        if 'accum_out' in kw:
            w.append(kw['accum_out'])
        return P.op(eng, lambda e: getattr(e, name)(*args, **kw), r=r, w=w)

    def mm(out, lhsT, rhs, start=True, stop=True):
        return P.op('pe', lambda e: e.matmul(out, lhsT, rhs, start=start, stop=stop, skip_group_check=True),
                    r=[lhsT, rhs], w=[out])

    def tp(out, in_, ident):
        return P.op('pe', lambda e: e.transpose(out, in_, ident), r=[in_, ident], w=[out])

    def dma(eng, out, in_, key):
        return dmas(eng, [(out, in_)], key)

    import os as _os
    _skip = set(_os.environ.get("KSKIP", "").split(","))

    def dmas(eng, pairs, key):
        if key in ("c0", "c1") and any(n and str(pairs[0][0].tensor.name).startswith(n) for n in _skip):
            return None
        def fn(e, sem):
            for o, i_ in pairs:
                e.dma_start(out=o, in_=i_).then_inc(sem, 16)
        r = [i_ for _, i_ in pairs]
        w = [o for o, _ in pairs]
        return P.op(eng, fn, r=r, w=w, dma=key, ndma=len(pairs))

    def act(out, in_, func, **kw):
        return I('act', 'activation', out, in_, func, **kw)

    kb = Bump(0, 18 * KB)
    ident = kb("ident", [128, 128], F32)
    ident_b = kb("ident_b", [128, 128], BF16)
    ones_b = kb("ones_b", [128, 128], BF16)
    ones_f = kb("ones_f", [128, 128], F32)
    ustr_b = kb("ustr_b", [128, 128], BF16)
    tri_b = kb("tri_b", [128, 128], BF16)
    iota_j = kb("iota_j", [128, CAP], F32)
    lnin_cols = kb("lnin_cols", [128, 32], F32)
    qg_col = kb("qg_col", [128, 6], F32)
    kvg_col = kb("kvg_col", [128, 4], F32)
    dw_cols = kb("dw_cols", [128, 8, 31], F32)
    dwb_col = kb("dwb_col", [128, 8], F32)
    cln_cols = kb("cln_cols", [128, 16], F32)
    b1_cols = kb("b1_cols", [128, NE, 32], F32)
    wr_sb = kb("wr_sb", [128, 16, NE], F32)
    brouter = kb("brouter", [128, NE], F32)
    hmask = kb("hmask", [128, 64], F32)
    rstd_col = kb("rstd_col", [128, 17], F32)
    Gt = kb("Gt", [128, 8, NE], F32)
    Ghl = kb("Ghl", [128, 8, NE, 2], BF16)
    posm = kb("posm", [128, 8, NE], F32)
    maskb = kb("maskb", [128, 8, NE], BF16)
    smallf = kb("smallf", [128, 256], F32)
    gate_g = kb("gate_g", [128, 2, 2], F32)
    GT_sb = kb("GT_sb", [128, 128], F32)

    dma('sp', ident[:], T.ident_d, 'c0')
    dma('pool', ident_b[:], T.ident_d, 'c1')
    dma('pool', ustr_b[:], T.ustr_d, 'c1')
    dma('pool', tri_b[:], T.tri_d, 'c1')
    dma('sp', iota_j[:], T.iota_d, 'c0')
    dma('sp', lnin_cols[:], T.lnin_cols_d, 'c0')
    dma('sp', qg_col[:], T.qg_col_d, 'c0')
    dma('sp', kvg_col[:], T.kvg_col_d, 'c0')
    dma('sp', dw_cols[:], T.dw_cols_d, 'c0')
    dma('sp', dwb_col[:], T.dwb_col_d, 'c0')
    dma('sp', cln_cols[:], T.cln_cols_d, 'c0')
    dma('sp', b1_cols[:], T.b1_cols_d, 'c0')
    dma('sp', wr_sb[:], T.w_router.rearrange("(kc p) n -> p kc n", p=128), 'c0')
    dma('sp', brouter[:], T.brouter_d, 'c0')
    dma('sp', hmask[:], T.halomask, 'c0')
    I('dve', 'memset', ones_b[:], 1.0)
    I('dve', 'memset', ones_f[:], 1.0)

    p2 = Bump(18 * KB, 85 * KB)
    ckvgT = p2("ckvgT", [128, 4, LKP], BF16)
    KpT = p2("KpT", [128, 2, LKP], BF16)
    cqgT = p2("cqgT", [128, 6, NOWN], BF16)
    rstd_kv = p2("rstd_kv", [128, 2048 + 128], F32)
    rstd_q = p2("rstd_q", [128, NOWN], F32)
    cosT = p2("cosT_sb", [64, LKP], F32)
    sinT = p2("sinT_sb", [64, LKP], F32)
    attnT = sb("attnT", [128, 8, NOWN], BF16, 175 * KB)
    convT = sb("convT", [128, 8, NOWN], BF16, 191 * KB)

    dma('sp', cosT[:], T.cosT_d, 'c0')
    dma('sp', sinT[:], T.sinT_d, 'c0')
    dma('pool', KpT[64:65, :, :], T.kbias_d.rearrange("(o s) n -> o s n", o=1), 'c1')

    stat_ctr = [0]

    def ln_stats(x_ap, rows, eps):
        k = stat_ctr[0] % 4
        stat_ctr[0] += 1
        base = k * 40
        st6 = smallf[:rows, base:base + 24]
        for j in range(4):
            I('dve', 'bn_stats', smallf[:rows, base + 6 * j: base + 6 * j + 6], x_ap[:, j * 512:(j + 1) * 512])
        mv = smallf[:rows, base + 24: base + 26]
        I('dve', 'bn_aggr', mv, st6)
        rstd = smallf[:rows, base + 26: base + 27]
        nmr = smallf[:rows, base + 27: base + 28]
        I('dve', 'tensor_scalar', rstd, smallf[:rows, base + 25: base + 26], eps, None, ALU.add)
        act(rstd, rstd, AF.Sqrt)
        I('dve', 'reciprocal', rstd, rstd)
        I('dve', 'scalar_tensor_tensor', nmr, smallf[:rows, base + 24: base + 25], -1.0, rstd, ALU.mult, ALU.mult)
        return rstd, nmr

    hT_b = [None] * 4
    hT_b[0] = sb("hT_b0", [128, 16, 512], BF16, 85 * KB)
    hT_b[3] = sb("hT_b3", [128, 16, 512], BF16, 101 * KB)
    hTs = sb("hTs", [128, 16, 256], BF16, 117 * KB)
    hT_b[1] = sb("hT_b1", [128, 16, 512], BF16, 125 * KB)
    hT_b[2] = sb("hT_b2", [128, 16, 512], BF16, 141 * KB)
    xs = [sb("xs%d" % i, [128, D], F32, (157 + 8 * i) * KB) for i in range(6)]

    xs_ctr = [0]

    def load_norm_tile(src_ap, rows):
        slot = xs[xs_ctr[0] % 6]
        k = xs_ctr[0] % 6
        xs_ctr[0] += 1
        dma('sp', slot[:rows, :], src_ap, 'xs%d' % k)
        rstd, nmr = ln_stats(slot[:rows, :], rows, LN_EPS)
        act(slot[:rows, :], slot[:rows, :], AF.Identity, bias=nmr, scale=rstd)
        return slot

    ev_ctr = [0]

    def evac_gb(out_ap, in_ap, c, rows=128):
        g = lnin_cols[:rows, c:c + 1]
        b = lnin_cols[:rows, 16 + c:17 + c]
        if ev_ctr[0] % 2 == 0:
            act(out_ap, in_ap, AF.Identity, bias=b, scale=g)
        else:
            I('dve', 'tensor_scalar', out_ap, in_ap, g, b, ALU.mult, ALU.add)
        ev_ctr[0] += 1

    _skipab = _os.environ.get('SKIPAB', '0') == '1'
    for g in range(4 if (stop >= 0 and not _skipab) else 0):
        slots = [load_norm_tile(T.xall[(4 * g + t) * 128:(4 * g + t + 1) * 128, :], 128) for t in range(4)]
        for cq in range(4 if _os.environ.get('A0CUT', '0') not in ('1',) else 0):
            for t in range(4):
                for c in range(4 * cq, 4 * cq + 4):
                    tp(ps[c % 4 + 4 * (cq % 2)][:, t * 128:(t + 1) * 128], slots[t][:, c * 128:(c + 1) * 128], ident[:])
            for c in range(4 * cq, 4 * cq + 4):
                evac_gb(hT_b[g][:, c, :], ps[c % 4 + 4 * (cq % 2)][:, :], c)
    _cut = _os.environ.get('A0CUT', '0')
    if stop >= 0 and _cut in ('0', '3', '4') and not _skipab:
        slots = [load_norm_tile(T.xall[2048:2176, :], 128), load_norm_tile(T.xhalo, 128)]
        for cq in range(4 if _cut != '4' else 0):
            for t in range(2):
                for c in range(4 * cq, 4 * cq + 4):
                    tp(ps[c % 4 + 4 * (cq % 2)][:, t * 128:(t + 1) * 128], slots[t][:, c * 128:(c + 1) * 128], ident[:])
            for c in range(4 * cq, 4 * cq + 4) if _cut != '3' else ():
                evac_gb(hTs[:, c, :], ps[c % 4 + 4 * (cq % 2)][:, 0:256], c)

    if debug:
        dbg['hT_b1'] = (hT_b[1], [128, 16, 512], BF16)
        dbg['hTs'] = (hTs, [128, 16, 256], BF16)

    wr = [sb("wr%d" % i, [128, 16, 256], BF16, (157 + 8 * i) * KB) for i in range(3)]
    sqt = [sb("sqt%d" % i, [128, 512], BF16, (181 + i) * KB) for i in range(2)]
    tmpA = sb("tmpA", [128, 512], F32, 183 * KB)
    tmpB = sb("tmpB", [128, 512], F32, 185 * KB)

    def wslice(w_ap, c0, n):
        return w_ap[:, c0:c0 + n].rearrange("(kc p) n -> p kc n", p=128)

    if stop >= 1 and not _skipab:
        dma('pool', wr[0][:], wslice(T.w_in, 768, 256), 'wr0')
        dma('pool', wr[1][:], wslice(T.w_in, 1024, 256), 'wr1')
        dmas('pool', [(wr[2][:, :, 0:64], wslice(T.w_in, 1280, 64)),
                      (wr[2][:, :, 64:96], wslice(T.w_in, 1312, 32)),
                      (wr[2][:, :, 96:128], wslice(T.w_in, 1280, 32))], 'wr2')
        blocks = [(hT_b[0], 0, 512), (hT_b[1], 512, 512), (hT_b[2], 1024, 512), (hT_b[3], 1536, 512), (None, 2048, 128)]
        bk = 0
        for (hb, col0, n) in blocks:
            def rhs(kc):
                return hb[:, kc, :] if hb is not None else hTs[:, kc, 0:128]
            pS = ps[2]
            for m in range(4):
                pA = ps[bk % 2]
                bk += 1
                for kc in range(16):
                    mm(pA[:, 0:n], wr[m // 2][:, kc, (m % 2) * 128:(m % 2) * 128 + 128], rhs(kc), start=(kc == 0), stop=(kc == 15))
                act(ckvgT[:, m, col0:col0 + n], pA[:, 0:n], AF.Copy, scale=kvg_col[:, m:m + 1])
                sq = sqt[m % 2]
                act(sq[:, 0:n], pA[:, 0:n], AF.Square)
                mm(pS[:, 0:n], ones_b[:], sq[:, 0:n], start=(m == 0), stop=(m == 3))
            I('dve', 'tensor_scalar', tmpA[:, 0:n], pS[:, 0:n], 1.0 / 512.0, RMS_EPS, ALU.mult, ALU.add)
            act(tmpA[:, 0:n], tmpA[:, 0:n], AF.Sqrt)
            I('dve', 'reciprocal', rstd_kv[:, col0:col0 + n], tmpA[:, 0:n])
            pK1 = ps[3]
            pK2 = ps[4]
            for kc in range(16):
                mm(pK1[0:64, 0:n], wr[2][:, kc, 0:64], rhs(kc), start=(kc == 0), stop=(kc == 15))
            for kc in range(16):
                mm(pK2[0:64, 0:n], wr[2][:, kc, 64:128], rhs(kc), start=(kc == 0), stop=(kc == 15))
            I('dve', 'tensor_tensor', tmpA[0:64, 0:n], pK1[0:64, 0:n], cosT[:, col0:col0 + n], ALU.mult)
            I('dve', 'tensor_tensor', tmpB[0:64, 0:n], pK2[0:64, 0:n], sinT[:, col0:col0 + n], ALU.mult)
            I('dve', 'tensor_tensor', KpT[0:64, 0, col0:col0 + n], tmpA[0:64, 0:n], tmpB[0:64, 0:n], ALU.add)
            I('pool', 'tensor_copy', KpT[0:64, 1, col0:col0 + n], KpT[0:64, 0, col0:col0 + n])
        for i in range(17):
            rows = 128
            pR = ps[5 + i % 2]
            tp(pR[:, 0:128], rstd_kv[:, i * 128:(i + 1) * 128], ident[:])
            I('dve', 'tensor_copy', rstd_col[:rows, i:i + 1], pR[:rows, 0:1])
        for j in range(3):
            dma('pool', wr[j][:], wslice(T.w_in, 256 * j, 256), 'wr%d' % j)
        for tb in range(2):
            hb = hT_b[1 + tb]
            pS = ps[2]
            for m in range(6):
                pA = ps[bk % 2]
                bk += 1
                for kc in range(16):
                    mm(pA[:, :], wr[m // 2][:, kc, (m % 2) * 128:(m % 2) * 128 + 128], hb[:, kc, :], start=(kc == 0), stop=(kc == 15))
                act(cqgT[:, m, tb * 512:(tb + 1) * 512], pA[:, :], AF.Copy, scale=qg_col[:, m:m + 1])
                sq = sqt[m % 2]
                act(sq[:, :], pA[:, :], AF.Square)
                mm(pS[:, :], ones_b[:], sq[:, :], start=(m == 0), stop=(m == 5))
            I('dve', 'tensor_scalar', tmpA[:, :], pS[:, :], 1.0 / 768.0, RMS_EPS, ALU.mult, ALU.add)
            act(tmpA[:, :], tmpA[:, :], AF.Sqrt)
            I('dve', 'reciprocal', rstd_q[:, tb * 512:(tb + 1) * 512], tmpA[:, :])
    if debug and stop >= 1:
        dbg['ckvgT'] = (ckvgT, [128, 4, LKP], BF16)
        dbg['KpT'] = (KpT, [128, 2, LKP], BF16)
        dbg['rstd_kv'] = (rstd_kv, [128, LKP], F32)
        dbg['cqgT'] = (cqgT, [128, 6, NOWN], BF16)

    if stop >= 2 and not _skipab:
        conv_out = sb("conv_out", [128, 8, 512], F32, 85 * KB)
        hc = [sb("hc%d" % i, [128, 544], F32, 101 * KB + i * 2176) for i in range(2)]
        sgt = [sb("sgt%d" % i, [128, 544], F32, 101 * KB + 4352 + i * 2176) for i in range(2)]
        cmean = sb("cmean", [128, 512], F32, 101 * KB + 8704)
        crstd = sb("crstd", [128, 512], F32, 101 * KB + 8704 + 2048)
        ctmp = [sb("ctmp%d" % i, [128, 512], F32, (187 + 2 * i) * KB) for i in range(2)]
        wctr = 0
        for blk in range(2):
            hb = hT_b[1 + blk]
            for c in range(8):
                slot = wr[wctr % 3]
                dmas('pool', [(slot[:, :, 0:128], wslice(T.w_in, 1344 + 128 * c, 128)),
                              (slot[:, :, 128:256], wslice(T.w_in, 2368 + 128 * c, 128))], 'wr%d' % (wctr % 3))
                wctr += 1
                pA, pG, pH = ps[0 + 3 * (c % 2)], ps[1 + 3 * (c % 2)], ps[2 + 3 * (c % 2)]
                for kc in range(16):
                    mm(pA[:, :], slot[:, kc, 0:128], hb[:, kc, :], start=(kc == 0), stop=(kc == 15))
                for kc in range(16):
                    mm(pG[:, :], slot[:, kc, 128:256], hb[:, kc, :], start=(kc == 0), stop=(kc == 15))
                hcols = slice(128 + 32 * blk, 128 + 32 * blk + 32)
                for kc in range(16):
                    mm(ps[6][:, 0:32], slot[:, kc, 0:128], hTs[:, kc, hcols], start=(kc == 0), stop=(kc == 15))
                for kc in range(16):
                    mm(ps[7][:, 0:32], slot[:, kc, 128:256], hTs[:, kc, hcols], start=(kc == 0), stop=(kc == 15))
                h_ = hc[c % 2]
                s_ = sgt[c % 2]
                act(s_[:, 32:544], pG[:, :], AF.Sigmoid)
                act(s_[:, 0:32], ps[7][:, 0:32], AF.Sigmoid)
                I('dve', 'tensor_tensor', h_[:, 32:544], pA[:, :], s_[:, 32:544], ALU.mult)
                I('dve', 'tensor_tensor', h_[:, 0:32], ps[6][:, 0:32], s_[:, 0:32], ALU.mult)
                I('dve', 'tensor_tensor', h_[:, 0:32], h_[:, 0:32], hmask[:, 32 * blk:32 * blk + 32], ALU.mult)
                ce = 'dve'
                co = conv_out[:, c, :]
                I(ce, 'tensor_scalar', co, h_[:, 2:514], dw_cols[:, c, 0:1], dwb_col[:, c:c + 1], ALU.mult, ALU.add)
                for k in range(1, 31):
                    I(ce, 'scalar_tensor_tensor', co, h_[:, 2 + k:514 + k], dw_cols[:, c, k:k + 1], co, ALU.mult, ALU.add)
            pM, pQ = ps[6], ps[7]
            for c in range(8):
                mm(pM[:, :], ones_f[:], conv_out[:, c, :], start=(c == 0), stop=(c == 7))
            for c in range(8):
                t_ = ctmp[c % 2]
                act(t_[:, :], conv_out[:, c, :], AF.Square)
                mm(pQ[:, :], ones_f[:], t_[:, :], start=(c == 0), stop=(c == 7))
            I('dve', 'tensor_scalar', cmean[:, :], pM[:, :], 1.0 / 1024.0, None, ALU.mult)
            I('dve', 'tensor_tensor', crstd[:, :], cmean[:, :], cmean[:, :], ALU.mult)
            I('dve', 'scalar_tensor_tensor', crstd[:, :], pQ[:, :], 1.0 / 1024.0, crstd[:, :], ALU.mult, ALU.subtract)
            I('dve', 'tensor_scalar', crstd[:, :], crstd[:, :], LN_EPS, None, ALU.add)
            act(crstd[:, :], crstd[:, :], AF.Sqrt)
            I('dve', 'reciprocal', crstd[:, :], crstd[:, :])
            for c in range(8):
                ce = 'dve' if c % 2 == 0 else 'pool'
                t_ = ctmp[c % 2]
                I(ce, 'tensor_tensor', t_[:, :], conv_out[:, c, :], cmean[:, :], ALU.subtract)
                I(ce, 'tensor_tensor', t_[:, :], t_[:, :], crstd[:, :], ALU.mult)
                act(convT[:, c, blk * 512:(blk + 1) * 512], t_[:, :], AF.Silu, bias=cln_cols[:, 8 + c:9 + c], scale=cln_cols[:, c:c + 1])
    if debug and stop >= 2:
        dbg['convT'] = (convT, [128, 8, NOWN], BF16)

    if stop >= 3 and not _skipab:
        b1 = Bump(85 * KB, 175 * KB)
        V_all = b1("V_all", [128, 17, 1024], BF16)
        w_uv_sb = b1("w_uv_sb", [128, 4, 1024], BF16)
        w_uk_sb = b1("w_uk_sb", [128, 4, 1024], BF16)
        wq = [b1("wq%d" % i, [128, 6, 256], BF16) for i in range(2)]
        KnT = [b1("KnT%d" % i, [128, LKP], BF16) for i in range(2)]
        QnT = [b1("QnT%d" % i, [128, NOWN], BF16) for i in range(2)]
        QpT = [b1("QpT%d" % i, [128, NOWN], BF16) for i in range(2)]
        PT = [b1("PT%d" % i, [128, 512], BF16) for i in range(4)]
        rs = [b1("rs%d" % i, [128, 512], F32) for i in range(2)]
        tq = [b1("tq%d" % i, [64, 512], F32) for i in range(2)]

        dma('pool', w_uv_sb[:], T.w_uv.rearrange("(kc p) n -> p kc n", p=128), 'wuv')
        dma('pool', w_uk_sb[:], T.w_uk.rearrange("(kc p) n -> p kc n", p=128), 'wuk')
        for i in range(2):
            I('pool', 'memset', QpT[i][64:65, :], 1.0)
        for i in range(17):
            rows = 128
            for nb in range(2):
                pV = ps[(2 * i + nb) % 2]
                for kc in range(4):
                    mm(pV[:rows, :], ckvgT[:, kc, i * 128:i * 128 + rows], w_uv_sb[:, kc, nb * 512:(nb + 1) * 512], start=(kc == 0), stop=(kc == 3))
                act(V_all[:rows, i, nb * 512:(nb + 1) * 512], pV[:rows, :], AF.Copy, scale=rstd_col[:rows, i:i + 1])
        ptc = 0
        sbank = 0
        for h in range(8):
            hb_ = h % 2
            wqh = wq[hb_]
            dmas('pool', [(wqh[:, :, 0:192], T.w_uq[:, h * 192:(h + 1) * 192].rearrange("(kc p) n -> p kc n", p=128)),
                          (wqh[:, :, 192:224], T.w_uq[:, h * 192 + 160:h * 192 + 192].rearrange("(kc p) n -> p kc n", p=128)),
                          (wqh[:, :, 224:256], T.w_uq[:, h * 192 + 128:h * 192 + 160].rearrange("(kc p) n -> p kc n", p=128))],
                 'wq%d' % hb_)
            kn, qn, qp = KnT[hb_], QnT[hb_], QpT[hb_]
            for tb in range(2):
                cols = slice(tb * 512, (tb + 1) * 512)
                pcols = slice(512 + tb * 512, 1024 + tb * 512)
                pA = ps[0]
                for kc in range(6):
                    mm(pA[:, :], wqh[:, kc, 0:128], cqgT[:, kc, cols], start=(kc == 0), stop=(kc == 5))
                I('dve', 'tensor_tensor', qn[:, cols], pA[:, :], rstd_q[:, cols], ALU.mult)
                pX, pXs = ps[1], ps[2]
                for kc in range(6):
                    mm(pX[0:64, :], wqh[:, kc, 128:192], cqgT[:, kc, cols], start=(kc == 0), stop=(kc == 5))
                for kc in range(6):
                    mm(pXs[0:64, :], wqh[:, kc, 192:256], cqgT[:, kc, cols], start=(kc == 0), stop=(kc == 5))
                I('dve', 'tensor_tensor', tq[0][:, :], pX[0:64, :], cosT[:, pcols], ALU.mult)
                I('dve', 'tensor_tensor', tq[1][:, :], pXs[0:64, :], sinT[:, pcols], ALU.mult)
                I('dve', 'tensor_tensor', tq[0][:, :], tq[0][:, :], tq[1][:, :], ALU.add)
                I('dve', 'tensor_tensor', qp[0:64, cols], tq[0][:, :], rstd_q[0:64, cols], ALU.mult)
            for (col0, n) in ((0, 512), (512, 512), (1024, 512), (1536, 512), (2048, 128)):
                pA = ps[3]
                for kc in range(4):
                    mm(pA[:, 0:n], w_uk_sb[:, kc, h * 128:(h + 1) * 128], ckvgT[:, kc, col0:col0 + n], start=(kc == 0), stop=(kc == 3))
                I('dve', 'tensor_tensor', kn[:, col0:col0 + n], pA[:, 0:n], rstd_kv[:, col0:col0 + n], ALU.mult)
            for s in range(2):
                q0 = s * 512
                if s == 0:
                    full = [0, 1, 2, 3]
                    diag = [4, 5, 6, 7]
                else:
                    full = [0, 1, 2, 3, 4, 5, 6, 7, 12, 13, 14, 15]
                    diag = [8, 9, 10, 11]
                tiles = [(kt, 128, 0) for kt in full] + [(16, 128, 0)] + [(kt, 128, r) for r, kt in enumerate(diag)]
                pO = ps[4 + (2 * h + s) % 2]
                pZ = ps[6 + (2 * h + s) % 2]
                for ti, (kt, rows, r) in enumerate(tiles):
                    qoff = 128 * r
                    n = 512 - qoff
                    kc0 = kt * 128
                    pS = ps[sbank % 3]
                    sbank += 1
                    mm(pS[:rows, 0:n], kn[:, kc0:kc0 + rows], qn[:, q0 + qoff:q0 + 512], start=True, stop=False)
                    mm(pS[:rows, 0:n], KpT[0:65, s, kc0:kc0 + rows], qp[0:65, q0 + qoff:q0 + 512], start=False, stop=True)
                    pt = PT[ptc % 4]
                    ptc += 1
                    act(pt[:rows, 0:n], pS[:rows, 0:n], AF.Exp, scale=QSCALE)
                    if ti > len(full):
                        I('pool', 'tensor_tensor', pt[:, 0:128], pt[:, 0:128], tri_b[:], ALU.mult)
                    first = (ti == 0)
                    last = (ti == len(tiles) - 1)
                    mm(pO[:, qoff:512], V_all[:rows, kt, h * 128:(h + 1) * 128], pt[:rows, 0:n], start=first, stop=last)
                    mm(pZ[:, qoff:512], ones_b[:rows, :], pt[:rows, 0:n], start=first, stop=last)
                r_ = rs[(2 * h + s) % 2]
                I('dve', 'reciprocal', r_[:, :], pZ[:, :])
                I('dve', 'tensor_tensor', attnT[:, h, q0:q0 + 512], pO[:, :], r_[:, :], ALU.mult)
    if debug and stop >= 3:
        dbg['attnT'] = (attnT, [128, 8, NOWN], BF16)

    acc = sb("acc", [128, 8, D], F32, 18 * KB)
    h1b = sb("h1b", [128, 8, D], BF16, 82 * KB)
    if stop >= 4 and not _skipab:
        wo = [sb("wo%d" % i, [128, 16, 256], BF16, (114 + 8 * i) * KB) for i in range(3)]
        xs2 = [sb("xs2_%d" % i, [128, D], F32, (138 + 8 * i) * KB) for i in range(2)]
        lng = sb("lng", [128, D], F32, 154 * KB)
        lnb = sb("lnb", [128, D], F32, 162 * KB)
        h1T = sb("h1T", [128, 16, 128], F32, 138 * KB)
        lgt = sb("lgt", [128, 4, NE], F32, 170 * KB)
        m8 = sb("m8", [128, 16], F32, 170 * KB + 512)
        dmas('sp', [(lng[:], T.lnin_bc_d[0]), (lnb[:], T.lnin_bc_d[1])], 'lnbc')
        for i in range(8):
            slot = xs2[i % 2]
            dma('sp', slot[:], T.xall[512 + i * 128:512 + (i + 1) * 128, :], 'xs2_%d' % (i % 2))
            rstd, nmr = ln_stats(slot[:], 128, LN_EPS)
            act(slot[:], slot[:], AF.Identity, bias=nmr, scale=rstd)
            I('dve', 'tensor_tensor', acc[:, i, :], slot[:], lng[:], ALU.mult)
            I('pool', 'tensor_tensor', acc[:, i, :], acc[:, i, :], lnb[:], ALU.add)
        for nb in range(8):
            slot = wo[nb % 3]
            dma('pool', slot[:], wslice(T.w_out, nb * 256, 256), 'wo%d' % (nb % 3))
            for i in range(8):
                pA = ps[(nb * 8 + i) % 4]
                for kc in range(16):
                    lhs = attnT[:, kc, i * 128:(i + 1) * 128] if kc < 8 else convT[:, kc - 8, i * 128:(i + 1) * 128]
                    mm(pA[:, 0:256], lhs, slot[:, kc, :], start=(kc == 0), stop=(kc == 15))
                I('dve', 'scalar_tensor_tensor', acc[:, i, nb * 256:(nb + 1) * 256], acc[:, i, nb * 256:(nb + 1) * 256], ALPHA, pA[:, 0:256], ALU.mult, ALU.add)
        dmas('sp', [(lng[:], T.ln1_bc_d[0]), (lnb[:], T.ln1_bc_d[1])], 'lnbc')
        _b2cut = _os.environ.get('B2CUT', 'Z')
        for i in range(8 if _b2cut != 'A' else 0):
            a_ = acc[:, i, :]
            rstd, nmr = ln_stats(a_, 128, LN_EPS)
            act(a_, a_, AF.Identity, bias=nmr, scale=rstd)
            I('dve', 'tensor_tensor', a_, a_, lng[:], ALU.mult)
            I('pool', 'tensor_tensor', a_, a_, lnb[:], ALU.add)
            I('pool', 'tensor_copy', h1b[:, i, :], a_)
            if _b2cut == 'B':
                continue
            for cq in range(4):
                pT = ps[cq % 2]
                for c in range(4 * cq, 4 * cq + 4):
                    tp(pT[:, (c % 4) * 128:(c % 4) * 128 + 128], acc[:, i, c * 128:(c + 1) * 128], ident[:])
                act(h1T[:, 4 * cq:4 * cq + 4, :], pT[:, :].rearrange("p (c t) -> p c t", c=4), AF.Copy)
            pL = ps[2]
            for c in range(16):
                mm(pL[:, 0:NE], h1T[:, c, :], wr_sb[:, c, :], start=(c == 0), stop=(c == 15))
            lg = lgt[:, 0, :]
            ex = lgt[:, 1, :]
            mk = lgt[:, 2, :]
            tm = lgt[:, 3, :]
            I('dve', 'tensor_tensor', lg, pL[:, 0:NE], brouter[:], ALU.add)
            I('dve', 'max', m8[:, 0:8], lg)
            I('dve', 'tensor_scalar', mk, lg, m8[:, 3:4], None, ALU.is_ge)
            I('dve', 'tensor_scalar', m8[:, 8:9], m8[:, 0:1], -1.0, None, ALU.mult)
            act(ex, lg, AF.Exp, bias=m8[:, 8:9], scale=1.0)
            I('dve', 'tensor_tensor', ex, ex, mk, ALU.mult)
            I('dve', 'tensor_reduce', m8[:, 9:10], ex, mybir.AxisListType.X, ALU.add)
            I('dve', 'reciprocal', m8[:, 10:11], m8[:, 9:10])
            I('dve', 'tensor_scalar', Gt[:, i, :], ex, m8[:, 10:11], None, ALU.mult)
            I('dve', 'tensor_copy', maskb[:, i, :], mk)
            I('dve', 'tensor_copy', Ghl[:, i, :, 0], Gt[:, i, :])
            I('dve', 'tensor_tensor', tm, Gt[:, i, :], Ghl[:, i, :, 0], ALU.subtract)
            I('dve', 'tensor_copy', Ghl[:, i, :, 1], tm)
            if _b2cut == 'C':
                continue
            pP = ps[3]
            for ip in range(i):
                mm(pP[:, 0:NE], ones_b[:], maskb[:, ip, :], start=(ip == 0), stop=False)
            mm(pP[:, 0:NE], ustr_b[:], maskb[:, i, :], start=(i == 0), stop=True)
            I('dve', 'scalar_tensor_tensor', posm[:, i, :], pP[:, 0:NE], 1.0, mk, ALU.add, ALU.mult)
            I('dve', 'tensor_scalar', posm[:, i, :], posm[:, i, :], -1.0, None, ALU.add)
            if _b2cut == 'D':
                continue
            I('dve', 'tensor_scalar', acc[:, i, :], acc[:, i, :], ALPHA, None, ALU.mult)
    if debug and stop >= 4:
        dbg['h1b'] = (h1b, [128, 8, D], BF16)
        dbg['Gt'] = (Gt, [128, 8, NE], F32)
        dbg['posm'] = (posm, [128, 8, NE], F32)
        dbg['acc0'] = (acc, [128, 8, D], F32)

    if stop >= 5:
        wring = [sb("wring%d" % i, [128, 16, 512], BF16, (114 + 16 * i) * KB) for i in range(3)]
        xgT = sb("xgT", [128, 16, CAP], BF16, 162 * KB)
        actT = sb("actT", [128, 16, CAP], BF16, 170 * KB)
        y_e = sb("y_e", [128, 2, D], BF16, 178 * KB)
        S_ = sb("S_", [128, 8, CAP], BF16, 186 * KB)
        ST = [sb("ST%d" % i, [128, 2, NOWN], BF16, (190 + 4 * i) * KB) for i in range(2)]
        tg = [sb("tg%d" % i, [128, CAP], F32, 198 * KB + i * 1024) for i in range(2)]
        ts_ = [sb("ts%d" % i, [128, CAP], F32, 200 * KB + i * 1024) for i in range(2)]
        tu = [sb("tu%d" % i, [128, CAP], F32, 202 * KB + i * 1024) for i in range(2)]
        b2row = [sb("b2row%d" % i, [1, 512], BF16, 204 * KB + i * 1024) for i in range(2)]
        ne_run = NE if stop >= 6 else 2
        wctr = [0]

        def wload(pairs_fn):
            k = wctr[0] % 3
            wctr[0] += 1
            slot = wring[k]
            dmas('pool', pairs_fn(slot), 'wring%d' % k)
            return slot

        def build_S(e):
            for i in range(8):
                I('dve', 'tensor_scalar', S_[:, i, :], iota_j[:], posm[:, i, e:e + 1], None, ALU.is_equal)

        def gather(e):
            for dp in range(8):
                pX = ps[6 + dp % 2]
                for half in range(2):
                    dc = 2 * dp + half
                    for i in range(8):
                        mm(pX[:, half * CAP:(half + 1) * CAP], h1b[:, i, dc * 128:(dc + 1) * 128], S_[:, i, :], start=(i == 0), stop=(i == 7))
                if dp % 2 == 0:
                    act(xgT[:, 2 * dp:2 * dp + 2, :], pX[:, :].rearrange("p (c t) -> p c t", c=2), AF.Copy)
                else:
                    I('dve', 'tensor_copy', xgT[:, 2 * dp:2 * dp + 2, :], pX[:, :].rearrange("p (c t) -> p c t", c=2))
            pg = ps[2]
            for jc in range(2):
                for i in range(8):
                    mm(pg[:, 2 * jc:2 * jc + 2], S_[:, i, jc * 128:(jc + 1) * 128], Ghl[:, i, e, :], start=(i == 0), stop=(i == 7))
            gg = gate_g[:, e % 2, :]
            I('dve', 'tensor_copy', smallf[:, 200:204], pg[:, 0:4])
            I('dve', 'tensor_tensor', gg, smallf[:, 200:204:2], smallf[:, 201:204:2], ALU.add)
            st = ST[e % 2]
            for jc in range(2):
                pT = ps[4 + jc][:].bitcast(BF16)
                for i in range(8):
                    tp(pT[:, i * 128:(i + 1) * 128], S_[:, i, jc * 128:(jc + 1) * 128], ident_b[:])
                act(st[:, jc, :], pT[:, 0:NOWN], AF.Copy)

        def mm1(e):
            for k4 in range(4):
                sg = wload(lambda s_: [(s_[:], T.w1[e][:, k4 * 512:(k4 + 1) * 512].rearrange("(kc p) n -> p kc n", p=128))])
                su = wload(lambda s_: [(s_[:], T.w1[e][:, DFF + k4 * 512:DFF + (k4 + 1) * 512].rearrange("(kc p) n -> p kc n", p=128))])
                for cc in range(4):
                    c = 4 * k4 + cc
                    pZ = ps[c % 2]
                    for kc in range(16):
                        mm(pZ[:, 0:CAP], sg[:, kc, cc * 128:(cc + 1) * 128], xgT[:, kc, :], start=(kc == 0), stop=(kc == 15))
                    for kc in range(16):
                        mm(pZ[:, CAP:2 * CAP], su[:, kc, cc * 128:(cc + 1) * 128], xgT[:, kc, :], start=(kc == 0), stop=(kc == 15))
                    g_, s2, u_ = tg[c % 2], ts_[c % 2], tu[c % 2]
                    _m1 = _os.environ.get('MM1CUT', 'z')
                    if _m1 == 'a':
                        continue
                    I('dve', 'tensor_scalar', g_[:, :], pZ[:, 0:CAP], b1_cols[:, e, c:c + 1], 7.0, ALU.add, ALU.min, xr=[pZ[:, :]])
                    act(s2[:, :], g_[:, :], AF.Sigmoid, scale=1.702)
                    I('dve', 'tensor_scalar', u_[:, :], pZ[:, CAP:2 * CAP], b1_cols[:, e, 16 + c:17 + c], 7.0, ALU.add, ALU.min)
                    if _m1 == 'b':
                        continue
                    I('dve', 'tensor_scalar', u_[:, :], u_[:, :], -7.0, 1.0, ALU.max, ALU.add)
                    I('pool', 'tensor_tensor', g_[:, :], g_[:, :], s2[:, :], ALU.mult)
                    I('pool', 'tensor_tensor', actT[:, c, :], g_[:, :], u_[:, :], ALU.mult)

        def mm2(e):
            for nb in range(4):
                slot = wload(lambda s_: [(s_[:], T.w2[e][:, nb * 512:(nb + 1) * 512].rearrange("(kc p) n -> p kc n", p=128))])
                br = b2row[nb % 2]
                dma('pool', br[0:1, :], T.b2_d[e:e + 1, nb * 512:(nb + 1) * 512], 'b2r%d' % (nb % 2))
                for jt in range(2):
                    pY = ps[2 + jt]
                    for kc in range(16):
                        mm(pY[:, :], actT[:, kc, jt * 128:(jt + 1) * 128], slot[:, kc, :], start=(kc == 0), stop=False)
                    mm(pY[:, :], ones_b[0:1, :], br[0:1, :], start=False, stop=True)
                    act(y_e[:, jt, nb * 512:(nb + 1) * 512], pY[:, :], AF.Copy, scale=gate_g[:, e % 2, jt:jt + 1])

        def scatter(e):
            st = ST[e % 2]
            k = 0
            for i in range(8):
                for nb in range(4):
                    pC = ps[4 + k % 2]
                    k += 1
                    mm(pC[:, :], st[:, 0, i * 128:(i + 1) * 128], y_e[:, 0, nb * 512:(nb + 1) * 512], start=True, stop=False)
                    mm(pC[:, :], st[:, 1, i * 128:(i + 1) * 128], y_e[:, 1, nb * 512:(nb + 1) * 512], start=False, stop=True)
                    I('dve', 'tensor_tensor', acc[:, i, nb * 512:(nb + 1) * 512], acc[:, i, nb * 512:(nb + 1) * 512], pC[:, :], ALU.add)

        _mc = _os.environ.get('MOECUT', '9')
        build_S(0)
        gather(0)
        for e in range(ne_run if _mc == '9' else 1):
            if _mc >= '2':
                mm1(e)
            if e + 1 < ne_run and _mc == '9':
                build_S(e + 1)
                gather(e + 1)
            if _mc >= '3':
                mm2(e)
            if _mc >= '4':
                scatter(e)

    if stop >= 4:
        lng2 = sb("lng2", [128, D], F32, 114 * KB)
        lnb2 = sb("lnb2", [128, D], F32, 122 * KB)
        dmas('sp', [(lng2[:], T.ln2_bc_d[0]), (lnb2[:], T.ln2_bc_d[1])], 'lnbc2')
        outs = []
        for i in range(8):
            a_ = acc[:, i, :]
            rstd, nmr = ln_stats(a_, 128, LN_EPS)
            act(a_, a_, AF.Identity, bias=nmr, scale=rstd)
            I('dve', 'tensor_tensor', a_, a_, lng2[:], ALU.mult)
            I('pool', 'tensor_tensor', a_, a_, lnb2[:], ALU.add)
            outs.append(dma('sp', out_d[i * 128:(i + 1) * 128, :], a_, 'out%d' % (i % 4)))
    else:
        outs = []
        outs.append(dma('sp', out_d[0:128, 0:256], smallf[:, 0:256], 'out0'))

    for name, (h, shape, dt) in dbg.items():
        dd = nc.dram_tensor("dbg_" + name, list(shape), dt, kind="ExternalOutput").ap()
        outs.append(dma('sp', dd, h[:], 'dbg'))

    fin = P.op('sp', lambda e: e.nop(), r=(), w=())
    P.ops[fin]['deps'] = set(outs)
    P.emit()
    nc._used_inputs = list(used_inputs)
    return nc


def _perm(s):
    return [0, 1, 2, 3] if s == 1 else [1, 0, 3, 2]


def _rope_tables():
    inv_freq = (1.0 / (10000.0 ** (np.arange(0, 64, 2, dtype=np.float32) / np.float32(64)))).astype(np.float32)
    pos = np.arange(LK, dtype=np.float32)
    freqs = (pos[:, None] * inv_freq[None, :]).astype(np.float32)
    emb = np.concatenate([freqs, freqs], axis=-1)
    cos = np.cos(emb).astype(np.float32)
    sin = np.sin(emb).astype(np.float32)
    sign = np.concatenate([-np.ones(32, np.float32), np.ones(32, np.float32)])
    return cos, sin * sign[None, :]


def make_in_maps(inputs, ne_alloc=NE):
    f = lambda a: np.ascontiguousarray(np.asarray(a, dtype=np.float32))
    x = f(inputs["x"])
    meta = f(inputs["meta_tokens"])
    cos, sinS = _rope_tables()
    shared = {}
    shared["ident"] = np.eye(128, dtype=np.float32)
    shared["tri"] = np.triu(np.ones((128, 128), np.float32))
    shared["ustrict"] = np.triu(np.ones((128, 128), np.float32), 1)
    shared["iota"] = np.ascontiguousarray(np.broadcast_to(np.arange(CAP, dtype=np.float32)[None, :], (128, CAP)))
    shared["w_in"] = f(inputs["w_in"][0])
    shared["w_uq"] = f(inputs["w_uq"][0])
    shared["w_uk"] = f(inputs["w_uk"][0])
    shared["w_uv"] = f(inputs["w_uv"][0])
    shared["w_out"] = f(inputs["w_out"][0])
    shared["w_router"] = f(inputs["w_router"][0])
    shared["w1"] = f(inputs["w_mlp1"][0][:ne_alloc])
    shared["w2"] = f(inputs["w_mlp2"][0][:ne_alloc])
    cols = lambda v, n: np.ascontiguousarray(f(v).reshape(n, 128).T)
    bc = lambda g, b: np.ascontiguousarray(np.stack([np.broadcast_to(f(g)[None, :], (128, D)), np.broadcast_to(f(b)[None, :], (128, D))]))
    shared["lnin_cols"] = np.ascontiguousarray(np.concatenate([cols(inputs["ln_in_g"], 16), cols(inputs["ln_in_b"], 16)], axis=1))
    shared["lnin_bc"] = bc(inputs["ln_in_g"], inputs["ln_in_b"])
    shared["ln1_bc"] = bc(inputs["ln1_g"][0], inputs["ln1_b"][0])
    shared["ln2_bc"] = bc(inputs["ln2_g"][0], inputs["ln2_b"][0])
    shared["qg_col"] = cols(inputs["q_norm_g"][0], 6)
    shared["kvg_col"] = cols(inputs["kv_norm_g"][0], 4)
    dw = f(inputs["conv_dw_w"][0])
    shared["dw_cols"] = np.ascontiguousarray(dw.reshape(31, 8, 128).transpose(2, 1, 0))
    shared["dwb_col"] = cols(inputs["conv_dw_b"][0], 8)
    shared["cln_cols"] = np.ascontiguousarray(np.concatenate([cols(inputs["conv_ln_g"][0], 8), cols(inputs["conv_ln_b"][0], 8)], axis=1))
    shared["b1_cols"] = np.ascontiguousarray(f(inputs["b_mlp1"][0]).reshape(NE, 32, 128).transpose(2, 0, 1))
    shared["b2"] = f(inputs["b_mlp2"][0])
    shared["brouter_bc"] = np.ascontiguousarray(np.broadcast_to(f(inputs["b_router"][0])[None, :], (128, NE)))
    in_maps = []
    for core in range(8):
        b, s = core // 2, core % 2
        perm = _perm(s)
        xb = x[b]
        rows = np.concatenate([xb[512 * tb:512 * (tb + 1)] for tb in perm] + [meta, np.zeros((112, D), np.float32)], axis=0)
        seqpos = np.concatenate([16 + 512 * tb + np.arange(512) for tb in perm] + [np.arange(16)])
        m = dict(shared)
        m["xall"] = np.ascontiguousarray(rows)
        pad = np.zeros((64, LKP - LK), np.float32)
        m["cosT"] = np.ascontiguousarray(np.concatenate([cos[seqpos].T, pad], axis=1))
        m["sinT"] = np.ascontiguousarray(np.concatenate([sinS[seqpos].T, pad], axis=1))
        kbias = np.zeros((2, LKP), np.float32)
        kbias[:, LK:] = NEG
        for slot, qpb in ((0, 1), (1, 2)):
            for pb in range(4):
                if pb != qpb and not (perm[pb] < perm[qpb]):
                    kbias[slot, 512 * pb:512 * (pb + 1)] = NEG
        m["kbias"] = kbias
        halo = np.zeros((128, D), np.float32)
        hm = np.zeros((64,), np.float32)
        seq = np.concatenate([meta, xb], axis=0)
        for blk, qpb in enumerate((1, 2)):
            t0 = 16 + 512 * perm[qpb]
            for j in range(32):
                sp_ = t0 - 32 + j
                if sp_ >= 0:
                    halo[32 * blk + j] = seq[sp_]
                    hm[32 * blk + j] = 1.0
        m["xhalo"] = halo
        m["halomask"] = np.ascontiguousarray(np.broadcast_to(hm[None, :], (128, 64)))
        in_maps.append(m)
    return in_maps


def assemble(results):
    out = np.zeros((4, SEQ, D), np.float32)
    for core in range(8):
        b, s = core // 2, core % 2
        perm = _perm(s)
        o = np.asarray(results[core]["out"], dtype=np.float32)
        out[b, 512 * perm[1]:512 * (perm[1] + 1)] = o[0:512]
        out[b, 512 * perm[2]:512 * (perm[2] + 1)] = o[512:1024]
    return out


def kernel(**inputs):
    nc = build_nc()
    in_maps = make_in_maps(inputs)
    in_maps = [{k: m[k] for k in nc._used_inputs} for m in in_maps]
    res = run_bass_kernel_spmd(nc, in_maps, core_ids=list(range(8)))
    return assemble(res.results)
```

```python
import math
from contextlib import ExitStack
import numpy as np
import concourse.bass as bass
import concourse.mybir as mybir
from concourse.bass_utils import run_bass_kernel_spmd

F32 = mybir.dt.float32
BF16 = mybir.dt.bfloat16
AF = mybir.ActivationFunctionType
ALU = mybir.AluOpType

D = 2048
SEQ = 2048
NMETA = 16
LK = SEQ + NMETA
LKP = SEQ + 128
NOWN = 1024
NE = 32
CAP = 256
DFF = 2048
ALPHA = 2.0 ** 0.25
LN_EPS = 1e-5
RMS_EPS = 1e-6
QSCALE = 1.0 / math.sqrt(192.0)
NEG = -30000.0
CELL = 32
SB_LIMIT = 212000
SB_BASE = 16640

ENGS = ['pe', 'act', 'dve', 'pool', 'sp']


def _is_ap(x):
    return hasattr(x, 'ap') and hasattr(x, 'tensor') and hasattr(x, 'offset')


class Prog:
    def __init__(self, nc):
        self.nc = nc
        self.ops = []
        self.vbase = {}
        ncell = (SB_LIMIT + 8 * 2048 + 4096) // CELL + 8
        self.last_w = np.full(ncell, -1, np.int64)
        self.last_r = {e: np.full(ncell, -1, np.int64) for e in ENGS}
        self.cache = {}
        self.dma_keys = {}
        self.last_dma = {}

    def reg(self, handle, vbase):
        self.vbase[handle.name] = vbase

    def cells(self, ap):
        sp = str(ap.space)
        if 'DRAM' in sp.upper():
            return None
        if 'PSUM' in sp.upper():
            key = ('psum', ap.tensor.name)
            c = self.cache.get(key)
            if c is None:
                base = self.vbase[ap.tensor.name]
                c = np.arange(base // CELL, (base + 2048) // CELL)
                self.cache[key] = c
            return c
        key = (ap.tensor.name, int(ap.offset), tuple(tuple(x) for x in ap.ap), str(ap.dtype))
        c = self.cache.get(key)
        if c is not None:
            return c
        es = mybir.dt.size(ap.dtype)
        dims = [tuple(x) for x in ap.ap]
        pstep = dims[0][0]
        off = int(ap.offset) % pstep if pstep > 0 else int(ap.offset)
        base = self.vbase[ap.tensor.name]
        idx = np.array([off], dtype=np.int64)
        free = dims[1:]
        if len(free) == 0:
            free = [(1, 1)]
        for step, cnt in free[:-1]:
            idx = (idx[:, None] + (np.arange(cnt, dtype=np.int64) * step)[None, :]).ravel()
        step, cnt = free[-1]
        if step == 1 or cnt == 1:
            lo = base + idx * es
            hi = base + (idx + cnt) * es - 1
        else:
            idx = (idx[:, None] + (np.arange(cnt, dtype=np.int64) * step)[None, :]).ravel()
            lo = base + idx * es
            hi = lo + es - 1
        lo = lo // CELL
        hi = hi // CELL
        n = int((hi - lo).max()) + 1
        c = np.unique((lo[:, None] + np.arange(n)[None, :]).clip(max=hi[:, None]))
        self.cache[key] = c
        return c

    def op(self, eng, fn, r=(), w=(), dma=None, ndma=0):
        i = len(self.ops)
        deps = set()
        rcells = [c for c in (self.cells(a) for a in r) if c is not None]
        wcells = [c for c in (self.cells(a) for a in w) if c is not None]
        raw = set()
        for c in rcells:
            u = np.unique(self.last_w[c])
            raw.update(int(x) for x in u if x >= 0)
        deps |= raw
        for c in wcells:
            u = np.unique(self.last_w[c])
            deps.update(int(x) for x in u if x >= 0)
            for e in ENGS:
                u = np.unique(self.last_r[e][c])
                deps.update(int(x) for x in u if x >= 0)
        keep = set()
        for d in deps:
            od = self.ops[d]
            if od['eng'] == eng and od['dma'] is None and dma is None:
                if eng == 'pe':
                    continue
                if d not in raw:
                    continue
            keep.add(d)
        if dma is not None and dma in self.last_dma:
            keep.add(self.last_dma[dma])
        for c in rcells:
            self.last_r[eng][c] = i
        for c in wcells:
            self.last_w[c] = i
            for e in ENGS:
                self.last_r[e][c] = -1
        self.ops.append(dict(eng=eng, fn=fn, deps=keep, dma=dma, ndma=ndma, signal=False))
        if dma is not None:
            self.last_dma[dma] = i
        return i

    def selfcheck(self, by_eng):
        ops = self.ops
        sem = {}
        pc = {e: 0 for e in ENGS}
        progress = True
        while progress:
            progress = False
            for e in ENGS:
                lst = by_eng[e]
                while pc[e] < len(lst):
                    o = lst[pc[e]]
                    ok = True
                    for d in o['deps']:
                        if 'sig' not in ops[d]:
                            raise RuntimeError("dep on unsignaled op %d" % d)
                        sn, val = ops[d]['sig']
                        if sem.get(sn, 0) < val:
                            ok = False
                            break
                    if not ok:
                        break
                    if o['dma'] is not None:
                        sn = 'd:' + o['dma']
                        sem[sn] = sem.get(sn, 0) + 16 * o['ndma']
                    elif o['signal']:
                        sn = 'e:' + o['eng']
                        sem[sn] = sem.get(sn, 0) + 1
                    pc[e] += 1
                    progress = True
        for e in ENGS:
            if pc[e] < len(by_eng[e]):
                o = by_eng[e][pc[e]]
                raise RuntimeError("deadlock: engine %s stuck at op %d/%d deps=%s" % (e, pc[e], len(by_eng[e]), [(d, ops[d]['eng'], ops[d].get('sig')) for d in o['deps']]))

    def emit(self):
        nc = self.nc
        ops = self.ops
        for o in ops:
            for d in o['deps']:
                ops[d]['signal'] = True
        cnt = {e: 0 for e in ENGS}
        dcnt = {}
        for o in ops:
            if o['dma'] is not None:
                k = o['dma']
                dcnt[k] = dcnt.get(k, 0) + 16 * o['ndma']
                o['sig'] = ('d:' + k, dcnt[k])
            elif o['signal']:
                cnt[o['eng']] += 1
                o['sig'] = ('e:' + o['eng'], cnt[o['eng']])
        semnames = ['e:' + e for e in ENGS] + ['d:' + k for k in dcnt]
        print('PROG tickets', cnt, 'dma', {k: v for k, v in dcnt.items() if v > 1000}, 'nops', len(ops), flush=True)
        by_eng = {e: [o for o in ops if o['eng'] == e] for e in ENGS}
        self.selfcheck(by_eng)
        with ExitStack() as st:
            sems = {}
            for j, n in enumerate(semnames):
                sems[n] = st.enter_context(nc.semaphore("s%d" % j))
            block = st.enter_context(nc.Block())

            def run(e, lst):
                waited = {}
                for o in lst:
                    need = {}
                    for d in o['deps']:
                        sn, val = ops[d]['sig']
                        if need.get(sn, 0) < val:
                            need[sn] = val
                    for sn, val in need.items():
                        if waited.get(sn, 0) < val:
                            e.wait_ge(sems[sn], val)
                            waited[sn] = val
                    if o['dma'] is not None:
                        o['fn'](e, sems['d:' + o['dma']])
                    else:
                        ins = o['fn'](e)
                        if o['signal']:
                            ins.then_inc(sems['e:' + o['eng']], 1)

            @block.tensor
            def _(e):
                run(e, by_eng['pe'])

            @block.scalar
            def _(e):
                run(e, by_eng['act'])

            @block.vector
            def _(e):
                run(e, by_eng['dve'])

            @block.gpsimd
            def _(e):
                run(e, by_eng['pool'])

            @block.sync
            def _(e):
                run(e, by_eng['sp'])


def build_nc(stop=99, debug=False, ne_alloc=NE):
    nc = bass.Bass("TRN2", target_bir_lowering=False)
    P = Prog(nc)

    def din(name, shape, dt=F32):
        return nc.dram_tensor(name, list(shape), dt, kind="ExternalInput").ap()

    _specs = {
        "xall": ("xall", [2048 + 128, D]),
        "xhalo": ("xhalo", [128, D]),
        "halomask": ("halomask", [128, 64]),
        "cosT_d": ("cosT", [64, LKP]),
        "sinT_d": ("sinT", [64, LKP]),
        "kbias_d": ("kbias", [2, LKP]),
        "ident_d": ("ident", [128, 128]),
        "tri_d": ("tri", [128, 128]),
        "ustr_d": ("ustrict", [128, 128]),
        "iota_d": ("iota", [128, CAP]),
        "w_in": ("w_in", [D, 3392]),
        "w_uq": ("w_uq", [768, 1536]),
        "w_uk": ("w_uk", [512, 1024]),
        "w_uv": ("w_uv", [512, 1024]),
        "w_out": ("w_out", [D, D]),
        "w_router": ("w_router", [D, NE]),
        "w1": ("w1", [ne_alloc, D, 2 * DFF]),
        "w2": ("w2", [ne_alloc, DFF, D]),
        "lnin_cols_d": ("lnin_cols", [128, 32]),
        "lnin_bc_d": ("lnin_bc", [2, 128, D]),
        "ln1_bc_d": ("ln1_bc", [2, 128, D]),
        "ln2_bc_d": ("ln2_bc", [2, 128, D]),
        "qg_col_d": ("qg_col", [128, 6]),
        "kvg_col_d": ("kvg_col", [128, 4]),
        "dw_cols_d": ("dw_cols", [128, 8, 31]),
        "dwb_col_d": ("dwb_col", [128, 8]),
        "cln_cols_d": ("cln_cols", [128, 16]),
        "b1_cols_d": ("b1_cols", [128, NE, 32]),
        "b2_d": ("b2", [NE, D]),
        "brouter_d": ("brouter_bc", [128, NE]),
    }
    used_inputs = []

    class _T:
        def __init__(self):
            self.c = {}

        def __getattr__(self, k):
            c = self.__dict__['c']
            if k not in c:
                n, shp = _specs[k]
                c[k] = din(n, shp)
                used_inputs.append(n)
            return c[k]

    T = _T()
    out_d = nc.dram_tensor("out", [NOWN, D], F32, kind="ExternalOutput").ap()
    dbg = {}

    def sb(name, shape, dt, at):
        nbytes = int(np.prod(shape[1:])) * mybir.dt.size(dt)
        assert at % 32 == 0, (name, at)
        assert at + nbytes <= SB_LIMIT, (name, at, nbytes)
        h = nc.alloc_sbuf_tensor_at(name, list(shape), dt, offset=at + SB_BASE)
        P.reg(h, at)
        return h

    class Bump:
        def __init__(self, lo, hi):
            self.p = lo
            self.hi = hi

        def __call__(self, name, shape, dt):
            nbytes = int(np.prod(shape[1:])) * mybir.dt.size(dt)
            nbytes = (nbytes + 31) // 32 * 32
            at = self.p
            assert at + nbytes <= self.hi, (name, at, nbytes, self.hi)
            self.p += nbytes
            return sb(name, shape, dt, at)

    KB = 1024
    ps = []
    for i in range(8):
        h = nc.alloc_psum_tensor("ps%d" % i, [128, 512], F32)
        P.reg(h, SB_LIMIT + 2048 * i)
        ps.append(h)

    def I(eng, name, *args, **kw):
        w = [args[0]]
        xr = kw.pop('xr', [])
        r = [a for a in args[1:] if _is_ap(a)] + [v for k, v in kw.items() if _is_ap(v) and k != 'accum_out'] + list(xr)
        if 'accum_out' in kw:
            w.append(kw['accum_out'])
        return P.op(eng, lambda e: getattr(e, name)(*args, **kw), r=r, w=w)

    def mm(out, lhsT, rhs, start=True, stop=True):
        return P.op('pe', lambda e: e.matmul(out, lhsT, rhs, start=start, stop=stop, skip_group_check=True),
                    r=[lhsT, rhs], w=[out])

    def tp(out, in_, ident):
        return P.op('pe', lambda e: e.transpose(out, in_, ident), r=[in_, ident], w=[out])

    def dma(eng, out, in_, key):
        return dmas(eng, [(out, in_)], key)

    import os as _os
    _skip = set(_os.environ.get("KSKIP", "").split(","))

    def dmas(eng, pairs, key):
        if key in ("c0", "c1") and any(n and str(pairs[0][0].tensor.name).startswith(n) for n in _skip):
            return None
        def fn(e, sem):
            for o, i_ in pairs:
                e.dma_start(out=o, in_=i_).then_inc(sem, 16)
        r = [i_ for _, i_ in pairs]
        w = [o for o, _ in pairs]
        return P.op(eng, fn, r=r, w=w, dma=key, ndma=len(pairs))

    def act(out, in_, func, **kw):
        return I('act', 'activation', out, in_, func, **kw)

    kb = Bump(0, 18 * KB)
    ident = kb("ident", [128, 128], F32)
    ident_b = kb("ident_b", [128, 128], BF16)
    ones_b = kb("ones_b", [128, 128], BF16)
    ones_f = kb("ones_f", [128, 128], F32)
    ustr_b = kb("ustr_b", [128, 128], BF16)
    tri_b = kb("tri_b", [128, 128], BF16)
    iota_j = kb("iota_j", [128, CAP], F32)
    lnin_cols = kb("lnin_cols", [128, 32], F32)
    qg_col = kb("qg_col", [128, 6], F32)
    kvg_col = kb("kvg_col", [128, 4], F32)
    dw_cols = kb("dw_cols", [128, 8, 31], F32)
    dwb_col = kb("dwb_col", [128, 8], F32)
    cln_cols = kb("cln_cols", [128, 16], F32)
    b1_cols = kb("b1_cols", [128, NE, 32], F32)
    wr_sb = kb("wr_sb", [128, 16, NE], F32)
    brouter = kb("brouter", [128, NE], F32)
    hmask = kb("hmask", [128, 64], F32)
    rstd_col = kb("rstd_col", [128, 17], F32)
    Gt = kb("Gt", [128, 8, NE], F32)
    Ghl = kb("Ghl", [128, 8, NE, 2], BF16)
    posm = kb("posm", [128, 8, NE], F32)
    maskb = kb("maskb", [128, 8, NE], BF16)
    smallf = kb("smallf", [128, 256], F32)
    gate_g = kb("gate_g", [128, 2, 2], F32)
    GT_sb = kb("GT_sb", [128, 128], F32)

    dma('sp', ident[:], T.ident_d, 'c0')
    dma('pool', ident_b[:], T.ident_d, 'c1')
    dma('pool', ustr_b[:], T.ustr_d, 'c1')
    dma('pool', tri_b[:], T.tri_d, 'c1')
    dma('sp', iota_j[:], T.iota_d, 'c0')
    dma('sp', lnin_cols[:], T.lnin_cols_d, 'c0')
    dma('sp', qg_col[:], T.qg_col_d, 'c0')
    dma('sp', kvg_col[:], T.kvg_col_d, 'c0')
    dma('sp', dw_cols[:], T.dw_cols_d, 'c0')
    dma('sp', dwb_col[:], T.dwb_col_d, 'c0')
    dma('sp', cln_cols[:], T.cln_cols_d, 'c0')
    dma('sp', b1_cols[:], T.b1_cols_d, 'c0')
    dma('sp', wr_sb[:], T.w_router.rearrange("(kc p) n -> p kc n", p=128), 'c0')
    dma('sp', brouter[:], T.brouter_d, 'c0')
    dma('sp', hmask[:], T.halomask, 'c0')
    I('dve', 'memset', ones_b[:], 1.0)
    I('dve', 'memset', ones_f[:], 1.0)

    p2 = Bump(18 * KB, 85 * KB)
    ckvgT = p2("ckvgT", [128, 4, LKP], BF16)
    KpT = p2("KpT", [128, 2, LKP], BF16)
    cqgT = p2("cqgT", [128, 6, NOWN], BF16)
    rstd_kv = p2("rstd_kv", [128, 2048 + 128], F32)
    rstd_q = p2("rstd_q", [128, NOWN], F32)
    cosT = p2("cosT_sb", [64, LKP], F32)
    sinT = p2("sinT_sb", [64, LKP], F32)
    attnT = sb("attnT", [128, 8, NOWN], BF16, 175 * KB)
    convT = sb("convT", [128, 8, NOWN], BF16, 191 * KB)

    dma('sp', cosT[:], T.cosT_d, 'c0')
    dma('sp', sinT[:], T.sinT_d, 'c0')
    dma('pool', KpT[64:65, :, :], T.kbias_d.rearrange("(o s) n -> o s n", o=1), 'c1')

    stat_ctr = [0]

    def ln_stats(x_ap, rows, eps):
        k = stat_ctr[0] % 4
        stat_ctr[0] += 1
        base = k * 40
        st6 = smallf[:rows, base:base + 24]
        for j in range(4):
            I('dve', 'bn_stats', smallf[:rows, base + 6 * j: base + 6 * j + 6], x_ap[:, j * 512:(j + 1) * 512])
        mv = smallf[:rows, base + 24: base + 26]
        I('dve', 'bn_aggr', mv, st6)
        rstd = smallf[:rows, base + 26: base + 27]
        nmr = smallf[:rows, base + 27: base + 28]
        I('dve', 'tensor_scalar', rstd, smallf[:rows, base + 25: base + 26], eps, None, ALU.add)
        act(rstd, rstd, AF.Sqrt)
        I('dve', 'reciprocal', rstd, rstd)
        I('dve', 'scalar_tensor_tensor', nmr, smallf[:rows, base + 24: base + 25], -1.0, rstd, ALU.mult, ALU.mult)
        return rstd, nmr

    hT_b = [None] * 4
    hT_b[0] = sb("hT_b0", [128, 16, 512], BF16, 85 * KB)
    hT_b[3] = sb("hT_b3", [128, 16, 512], BF16, 101 * KB)
    hTs = sb("hTs", [128, 16, 256], BF16, 117 * KB)
    hT_b[1] = sb("hT_b1", [128, 16, 512], BF16, 125 * KB)
    hT_b[2] = sb("hT_b2", [128, 16, 512], BF16, 141 * KB)
    xs = [sb("xs%d" % i, [128, D], F32, (157 + 8 * i) * KB) for i in range(6)]

    xs_ctr = [0]

    def load_norm_tile(src_ap, rows):
        slot = xs[xs_ctr[0] % 6]
        k = xs_ctr[0] % 6
        xs_ctr[0] += 1
        dma('sp', slot[:rows, :], src_ap, 'xs%d' % k)
        rstd, nmr = ln_stats(slot[:rows, :], rows, LN_EPS)
        act(slot[:rows, :], slot[:rows, :], AF.Identity, bias=nmr, scale=rstd)
        return slot

    ev_ctr = [0]

    def evac_gb(out_ap, in_ap, c, rows=128):
        g = lnin_cols[:rows, c:c + 1]
        b = lnin_cols[:rows, 16 + c:17 + c]
        if ev_ctr[0] % 2 == 0:
            act(out_ap, in_ap, AF.Identity, bias=b, scale=g)
        else:
            I('dve', 'tensor_scalar', out_ap, in_ap, g, b, ALU.mult, ALU.add)
        ev_ctr[0] += 1

    _skipab = _os.environ.get('SKIPAB', '0') == '1'
    for g in range(4 if (stop >= 0 and not _skipab) else 0):
        slots = [load_norm_tile(T.xall[(4 * g + t) * 128:(4 * g + t + 1) * 128, :], 128) for t in range(4)]
        for cq in range(4 if _os.environ.get('A0CUT', '0') not in ('1',) else 0):
            for t in range(4):
                for c in range(4 * cq, 4 * cq + 4):
                    tp(ps[c % 4 + 4 * (cq % 2)][:, t * 128:(t + 1) * 128], slots[t][:, c * 128:(c + 1) * 128], ident[:])
            for c in range(4 * cq, 4 * cq + 4):
                evac_gb(hT_b[g][:, c, :], ps[c % 4 + 4 * (cq % 2)][:, :], c)
    _cut = _os.environ.get('A0CUT', '0')
    if stop >= 0 and _cut in ('0', '3', '4') and not _skipab:
        slots = [load_norm_tile(T.xall[2048:2176, :], 128), load_norm_tile(T.xhalo, 128)]
        for cq in range(4 if _cut != '4' else 0):
            for t in range(2):
                for c in range(4 * cq, 4 * cq + 4):
                    tp(ps[c % 4 + 4 * (cq % 2)][:, t * 128:(t + 1) * 128], slots[t][:, c * 128:(c + 1) * 128], ident[:])
            for c in range(4 * cq, 4 * cq + 4) if _cut != '3' else ():
                evac_gb(hTs[:, c, :], ps[c % 4 + 4 * (cq % 2)][:, 0:256], c)

    if debug:
        dbg['hT_b1'] = (hT_b[1], [128, 16, 512], BF16)
        dbg['hTs'] = (hTs, [128, 16, 256], BF16)

    wr = [sb("wr%d" % i, [128, 16, 256], BF16, (157 + 8 * i) * KB) for i in range(3)]
    sqt = [sb("sqt%d" % i, [128, 512], BF16, (181 + i) * KB) for i in range(2)]
    tmpA = sb("tmpA", [128, 512], F32, 183 * KB)
    tmpB = sb("tmpB", [128, 512], F32, 185 * KB)

    def wslice(w_ap, c0, n):
        return w_ap[:, c0:c0 + n].rearrange("(kc p) n -> p kc n", p=128)

    if stop >= 1 and not _skipab:
        dma('pool', wr[0][:], wslice(T.w_in, 768, 256), 'wr0')
        dma('pool', wr[1][:], wslice(T.w_in, 1024, 256), 'wr1')
        dmas('pool', [(wr[2][:, :, 0:64], wslice(T.w_in, 1280, 64)),
                      (wr[2][:, :, 64:96], wslice(T.w_in, 1312, 32)),
                      (wr[2][:, :, 96:128], wslice(T.w_in, 1280, 32))], 'wr2')
        blocks = [(hT_b[0], 0, 512), (hT_b[1], 512, 512), (hT_b[2], 1024, 512), (hT_b[3], 1536, 512), (None, 2048, 128)]
        bk = 0
        for (hb, col0, n) in blocks:
            def rhs(kc):
                return hb[:, kc, :] if hb is not None else hTs[:, kc, 0:128]
            pS = ps[2]
            for m in range(4):
                pA = ps[bk % 2]
                bk += 1
                for kc in range(16):
                    mm(pA[:, 0:n], wr[m // 2][:, kc, (m % 2) * 128:(m % 2) * 128 + 128], rhs(kc), start=(kc == 0), stop=(kc == 15))
                act(ckvgT[:, m, col0:col0 + n], pA[:, 0:n], AF.Copy, scale=kvg_col[:, m:m + 1])
                sq = sqt[m % 2]
                act(sq[:, 0:n], pA[:, 0:n], AF.Square)
                mm(pS[:, 0:n], ones_b[:], sq[:, 0:n], start=(m == 0), stop=(m == 3))
            I('dve', 'tensor_scalar', tmpA[:, 0:n], pS[:, 0:n], 1.0 / 512.0, RMS_EPS, ALU.mult, ALU.add)
            act(tmpA[:, 0:n], tmpA[:, 0:n], AF.Sqrt)
            I('dve', 'reciprocal', rstd_kv[:, col0:col0 + n], tmpA[:, 0:n])
            pK1 = ps[3]
            pK2 = ps[4]
            for kc in range(16):
                mm(pK1[0:64, 0:n], wr[2][:, kc, 0:64], rhs(kc), start=(kc == 0), stop=(kc == 15))
            for kc in range(16):
                mm(pK2[0:64, 0:n], wr[2][:, kc, 64:128], rhs(kc), start=(kc == 0), stop=(kc == 15))
            I('dve', 'tensor_tensor', tmpA[0:64, 0:n], pK1[0:64, 0:n], cosT[:, col0:col0 + n], ALU.mult)
            I('dve', 'tensor_tensor', tmpB[0:64, 0:n], pK2[0:64, 0:n], sinT[:, col0:col0 + n], ALU.mult)
            I('dve', 'tensor_tensor', KpT[0:64, 0, col0:col0 + n], tmpA[0:64, 0:n], tmpB[0:64, 0:n], ALU.add)
            I('pool', 'tensor_copy', KpT[0:64, 1, col0:col0 + n], KpT[0:64, 0, col0:col0 + n])
        for i in range(17):
            rows = 128
            pR = ps[5 + i % 2]
            tp(pR[:, 0:128], rstd_kv[:, i * 128:(i + 1) * 128], ident[:])
            I('dve', 'tensor_copy', rstd_col[:rows, i:i + 1], pR[:rows, 0:1])
        for j in range(3):
            dma('pool', wr[j][:], wslice(T.w_in, 256 * j, 256), 'wr%d' % j)
        for tb in range(2):
            hb = hT_b[1 + tb]
            pS = ps[2]
            for m in range(6):
                pA = ps[bk % 2]
                bk += 1
                for kc in range(16):
                    mm(pA[:, :], wr[m // 2][:, kc, (m % 2) * 128:(m % 2) * 128 + 128], hb[:, kc, :], start=(kc == 0), stop=(kc == 15))
                act(cqgT[:, m, tb * 512:(tb + 1) * 512], pA[:, :], AF.Copy, scale=qg_col[:, m:m + 1])
                sq = sqt[m % 2]
                act(sq[:, :], pA[:, :], AF.Square)
                mm(pS[:, :], ones_b[:], sq[:, :], start=(m == 0), stop=(m == 5))
            I('dve', 'tensor_scalar', tmpA[:, :], pS[:, :], 1.0 / 768.0, RMS_EPS, ALU.mult, ALU.add)
            act(tmpA[:, :], tmpA[:, :], AF.Sqrt)
            I('dve', 'reciprocal', rstd_q[:, tb * 512:(tb + 1) * 512], tmpA[:, :])
    if debug and stop >= 1:
        dbg['ckvgT'] = (ckvgT, [128, 4, LKP], BF16)
        dbg['KpT'] = (KpT, [128, 2, LKP], BF16)
        dbg['rstd_kv'] = (rstd_kv, [128, LKP], F32)
        dbg['cqgT'] = (cqgT, [128, 6, NOWN], BF16)

    if stop >= 2 and not _skipab:
        conv_out = sb("conv_out", [128, 8, 512], F32, 85 * KB)
        hc = [sb("hc%d" % i, [128, 544], F32, 101 * KB + i * 2176) for i in range(2)]
        sgt = [sb("sgt%d" % i, [128, 544], F32, 101 * KB + 4352 + i * 2176) for i in range(2)]
        cmean = sb("cmean", [128, 512], F32, 101 * KB + 8704)
        crstd = sb("crstd", [128, 512], F32, 101 * KB + 8704 + 2048)
        ctmp = [sb("ctmp%d" % i, [128, 512], F32, (187 + 2 * i) * KB) for i in range(2)]
        wctr = 0
        for blk in range(2):
            hb = hT_b[1 + blk]
            for c in range(8):
                slot = wr[wctr % 3]
                dmas('pool', [(slot[:, :, 0:128], wslice(T.w_in, 1344 + 128 * c, 128)),
                              (slot[:, :, 128:256], wslice(T.w_in, 2368 + 128 * c, 128))], 'wr%d' % (wctr % 3))
                wctr += 1
                pA, pG, pH = ps[0 + 3 * (c % 2)], ps[1 + 3 * (c % 2)], ps[2 + 3 * (c % 2)]
                for kc in range(16):
                    mm(pA[:, :], slot[:, kc, 0:128], hb[:, kc, :], start=(kc == 0), stop=(kc == 15))
                for kc in range(16):
                    mm(pG[:, :], slot[:, kc, 128:256], hb[:, kc, :], start=(kc == 0), stop=(kc == 15))
                hcols = slice(128 + 32 * blk, 128 + 32 * blk + 32)
                for kc in range(16):
                    mm(ps[6][:, 0:32], slot[:, kc, 0:128], hTs[:, kc, hcols], start=(kc == 0), stop=(kc == 15))
                for kc in range(16):
                    mm(ps[7][:, 0:32], slot[:, kc, 128:256], hTs[:, kc, hcols], start=(kc == 0), stop=(kc == 15))
                h_ = hc[c % 2]
                s_ = sgt[c % 2]
                act(s_[:, 32:544], pG[:, :], AF.Sigmoid)
                act(s_[:, 0:32], ps[7][:, 0:32], AF.Sigmoid)
                I('dve', 'tensor_tensor', h_[:, 32:544], pA[:, :], s_[:, 32:544], ALU.mult)
                I('dve', 'tensor_tensor', h_[:, 0:32], ps[6][:, 0:32], s_[:, 0:32], ALU.mult)
                I('dve', 'tensor_tensor', h_[:, 0:32], h_[:, 0:32], hmask[:, 32 * blk:32 * blk + 32], ALU.mult)
                ce = 'dve'
                co = conv_out[:, c, :]
                I(ce, 'tensor_scalar', co, h_[:, 2:514], dw_cols[:, c, 0:1], dwb_col[:, c:c + 1], ALU.mult, ALU.add)
                for k in range(1, 31):
                    I(ce, 'scalar_tensor_tensor', co, h_[:, 2 + k:514 + k], dw_cols[:, c, k:k + 1], co, ALU.mult, ALU.add)
            pM, pQ = ps[6], ps[7]
            for c in range(8):
                mm(pM[:, :], ones_f[:], conv_out[:, c, :], start=(c == 0), stop=(c == 7))
            for c in range(8):
                t_ = ctmp[c % 2]
                act(t_[:, :], conv_out[:, c, :], AF.Square)
                mm(pQ[:, :], ones_f[:], t_[:, :], start=(c == 0), stop=(c == 7))
            I('dve', 'tensor_scalar', cmean[:, :], pM[:, :], 1.0 / 1024.0, None, ALU.mult)
            I('dve', 'tensor_tensor', crstd[:, :], cmean[:, :], cmean[:, :], ALU.mult)
            I('dve', 'scalar_tensor_tensor', crstd[:, :], pQ[:, :], 1.0 / 1024.0, crstd[:, :], ALU.mult, ALU.subtract)
            I('dve', 'tensor_scalar', crstd[:, :], crstd[:, :], LN_EPS, None, ALU.add)
            act(crstd[:, :], crstd[:, :], AF.Sqrt)
            I('dve', 'reciprocal', crstd[:, :], crstd[:, :])
            for c in range(8):
                ce = 'dve' if c % 2 == 0 else 'pool'
                t_ = ctmp[c % 2]
                I(ce, 'tensor_tensor', t_[:, :], conv_out[:, c, :], cmean[:, :], ALU.subtract)
                I(ce, 'tensor_tensor', t_[:, :], t_[:, :], crstd[:, :], ALU.mult)
                act(convT[:, c, blk * 512:(blk + 1) * 512], t_[:, :], AF.Silu, bias=cln_cols[:, 8 + c:9 + c], scale=cln_cols[:, c:c + 1])
    if debug and stop >= 2:
        dbg['convT'] = (convT, [128, 8, NOWN], BF16)

    if stop >= 3 and not _skipab:
        b1 = Bump(85 * KB, 175 * KB)
        V_all = b1("V_all", [128, 17, 1024], BF16)
        w_uv_sb = b1("w_uv_sb", [128, 4, 1024], BF16)
        w_uk_sb = b1("w_uk_sb", [128, 4, 1024], BF16)
        wq = [b1("wq%d" % i, [128, 6, 256], BF16) for i in range(2)]
        KnT = [b1("KnT%d" % i, [128, LKP], BF16) for i in range(2)]
        QnT = [b1("QnT%d" % i, [128, NOWN], BF16) for i in range(2)]
        QpT = [b1("QpT%d" % i, [128, NOWN], BF16) for i in range(2)]
        PT = [b1("PT%d" % i, [128, 512], BF16) for i in range(4)]
        rs = [b1("rs%d" % i, [128, 512], F32) for i in range(2)]
        tq = [b1("tq%d" % i, [64, 512], F32) for i in range(2)]

        dma('pool', w_uv_sb[:], T.w_uv.rearrange("(kc p) n -> p kc n", p=128), 'wuv')
        dma('pool', w_uk_sb[:], T.w_uk.rearrange("(kc p) n -> p kc n", p=128), 'wuk')
        for i in range(2):
            I('pool', 'memset', QpT[i][64:65, :], 1.0)
        for i in range(17):
            rows = 128
            for nb in range(2):
                pV = ps[(2 * i + nb) % 2]
                for kc in range(4):
                    mm(pV[:rows, :], ckvgT[:, kc, i * 128:i * 128 + rows], w_uv_sb[:, kc, nb * 512:(nb + 1) * 512], start=(kc == 0), stop=(kc == 3))
                act(V_all[:rows, i, nb * 512:(nb + 1) * 512], pV[:rows, :], AF.Copy, scale=rstd_col[:rows, i:i + 1])
        ptc = 0
        sbank = 0
        for h in range(8):
            hb_ = h % 2
            wqh = wq[hb_]
            dmas('pool', [(wqh[:, :, 0:192], T.w_uq[:, h * 192:(h + 1) * 192].rearrange("(kc p) n -> p kc n", p=128)),
                          (wqh[:, :, 192:224], T.w_uq[:, h * 192 + 160:h * 192 + 192].rearrange("(kc p) n -> p kc n", p=128)),
                          (wqh[:, :, 224:256], T.w_uq[:, h * 192 + 128:h * 192 + 160].rearrange("(kc p) n -> p kc n", p=128))],
                 'wq%d' % hb_)
            kn, qn, qp = KnT[hb_], QnT[hb_], QpT[hb_]
            for tb in range(2):
                cols = slice(tb * 512, (tb + 1) * 512)
                pcols = slice(512 + tb * 512, 1024 + tb * 512)
                pA = ps[0]
                for kc in range(6):
                    mm(pA[:, :], wqh[:, kc, 0:128], cqgT[:, kc, cols], start=(kc == 0), stop=(kc == 5))
                I('dve', 'tensor_tensor', qn[:, cols], pA[:, :], rstd_q[:, cols], ALU.mult)
                pX, pXs = ps[1], ps[2]
                for kc in range(6):
                    mm(pX[0:64, :], wqh[:, kc, 128:192], cqgT[:, kc, cols], start=(kc == 0), stop=(kc == 5))
                for kc in range(6):
                    mm(pXs[0:64, :], wqh[:, kc, 192:256], cqgT[:, kc, cols], start=(kc == 0), stop=(kc == 5))
                I('dve', 'tensor_tensor', tq[0][:, :], pX[0:64, :], cosT[:, pcols], ALU.mult)
                I('dve', 'tensor_tensor', tq[1][:, :], pXs[0:64, :], sinT[:, pcols], ALU.mult)
                I('dve', 'tensor_tensor', tq[0][:, :], tq[0][:, :], tq[1][:, :], ALU.add)
                I('dve', 'tensor_tensor', qp[0:64, cols], tq[0][:, :], rstd_q[0:64, cols], ALU.mult)
            for (col0, n) in ((0, 512), (512, 512), (1024, 512), (1536, 512), (2048, 128)):
                pA = ps[3]
                for kc in range(4):
                    mm(pA[:, 0:n], w_uk_sb[:, kc, h * 128:(h + 1) * 128], ckvgT[:, kc, col0:col0 + n], start=(kc == 0), stop=(kc == 3))
                I('dve', 'tensor_tensor', kn[:, col0:col0 + n], pA[:, 0:n], rstd_kv[:, col0:col0 + n], ALU.mult)
            for s in range(2):
                q0 = s * 512
                if s == 0:
                    full = [0, 1, 2, 3]
                    diag = [4, 5, 6, 7]
                else:
                    full = [0, 1, 2, 3, 4, 5, 6, 7, 12, 13, 14, 15]
                    diag = [8, 9, 10, 11]
                tiles = [(kt, 128, 0) for kt in full] + [(16, 128, 0)] + [(kt, 128, r) for r, kt in enumerate(diag)]
                pO = ps[4 + (2 * h + s) % 2]
                pZ = ps[6 + (2 * h + s) % 2]
                for ti, (kt, rows, r) in enumerate(tiles):
                    qoff = 128 * r
                    n = 512 - qoff
                    kc0 = kt * 128
                    pS = ps[sbank % 3]
                    sbank += 1
                    mm(pS[:rows, 0:n], kn[:, kc0:kc0 + rows], qn[:, q0 + qoff:q0 + 512], start=True, stop=False)
                    mm(pS[:rows, 0:n], KpT[0:65, s, kc0:kc0 + rows], qp[0:65, q0 + qoff:q0 + 512], start=False, stop=True)
                    pt = PT[ptc % 4]
                    ptc += 1
                    act(pt[:rows, 0:n], pS[:rows, 0:n], AF.Exp, scale=QSCALE)
                    if ti > len(full):
                        I('pool', 'tensor_tensor', pt[:, 0:128], pt[:, 0:128], tri_b[:], ALU.mult)
                    first = (ti == 0)
                    last = (ti == len(tiles) - 1)
                    mm(pO[:, qoff:512], V_all[:rows, kt, h * 128:(h + 1) * 128], pt[:rows, 0:n], start=first, stop=last)
                    mm(pZ[:, qoff:512], ones_b[:rows, :], pt[:rows, 0:n], start=first, stop=last)
                r_ = rs[(2 * h + s) % 2]
                I('dve', 'reciprocal', r_[:, :], pZ[:, :])
                I('dve', 'tensor_tensor', attnT[:, h, q0:q0 + 512], pO[:, :], r_[:, :], ALU.mult)
    if debug and stop >= 3:
        dbg['attnT'] = (attnT, [128, 8, NOWN], BF16)

    acc = sb("acc", [128, 8, D], F32, 18 * KB)
    h1b = sb("h1b", [128, 8, D], BF16, 82 * KB)
    if stop >= 4 and not _skipab:
        wo = [sb("wo%d" % i, [128, 16, 256], BF16, (114 + 8 * i) * KB) for i in range(3)]
        xs2 = [sb("xs2_%d" % i, [128, D], F32, (138 + 8 * i) * KB) for i in range(2)]
        lng = sb("lng", [128, D], F32, 154 * KB)
        lnb = sb("lnb", [128, D], F32, 162 * KB)
        h1T = sb("h1T", [128, 16, 128], F32, 138 * KB)
        lgt = sb("lgt", [128, 4, NE], F32, 170 * KB)
        m8 = sb("m8", [128, 16], F32, 170 * KB + 512)
        dmas('sp', [(lng[:], T.lnin_bc_d[0]), (lnb[:], T.lnin_bc_d[1])], 'lnbc')
        for i in range(8):
            slot = xs2[i % 2]
            dma('sp', slot[:], T.xall[512 + i * 128:512 + (i + 1) * 128, :], 'xs2_%d' % (i % 2))
            rstd, nmr = ln_stats(slot[:], 128, LN_EPS)
            act(slot[:], slot[:], AF.Identity, bias=nmr, scale=rstd)
            I('dve', 'tensor_tensor', acc[:, i, :], slot[:], lng[:], ALU.mult)
            I('pool', 'tensor_tensor', acc[:, i, :], acc[:, i, :], lnb[:], ALU.add)
        for nb in range(8):
            slot = wo[nb % 3]
            dma('pool', slot[:], wslice(T.w_out, nb * 256, 256), 'wo%d' % (nb % 3))
            for i in range(8):
                pA = ps[(nb * 8 + i) % 4]
                for kc in range(16):
                    lhs = attnT[:, kc, i * 128:(i + 1) * 128] if kc < 8 else convT[:, kc - 8, i * 128:(i + 1) * 128]
                    mm(pA[:, 0:256], lhs, slot[:, kc, :], start=(kc == 0), stop=(kc == 15))
                I('dve', 'scalar_tensor_tensor', acc[:, i, nb * 256:(nb + 1) * 256], acc[:, i, nb * 256:(nb + 1) * 256], ALPHA, pA[:, 0:256], ALU.mult, ALU.add)
        dmas('sp', [(lng[:], T.ln1_bc_d[0]), (lnb[:], T.ln1_bc_d[1])], 'lnbc')
        _b2cut = _os.environ.get('B2CUT', 'Z')
        for i in range(8 if _b2cut != 'A' else 0):
            a_ = acc[:, i, :]
            rstd, nmr = ln_stats(a_, 128, LN_EPS)
            act(a_, a_, AF.Identity, bias=nmr, scale=rstd)
            I('dve', 'tensor_tensor', a_, a_, lng[:], ALU.mult)
            I('pool', 'tensor_tensor', a_, a_, lnb[:], ALU.add)
            I('pool', 'tensor_copy', h1b[:, i, :], a_)
            if _b2cut == 'B':
                continue
            for cq in range(4):
                pT = ps[cq % 2]
                for c in range(4 * cq, 4 * cq + 4):
                    tp(pT[:, (c % 4) * 128:(c % 4) * 128 + 128], acc[:, i, c * 128:(c + 1) * 128], ident[:])
                act(h1T[:, 4 * cq:4 * cq + 4, :], pT[:, :].rearrange("p (c t) -> p c t", c=4), AF.Copy)
            pL = ps[2]
            for c in range(16):
                mm(pL[:, 0:NE], h1T[:, c, :], wr_sb[:, c, :], start=(c == 0), stop=(c == 15))
            lg = lgt[:, 0, :]
            ex = lgt[:, 1, :]
            mk = lgt[:, 2, :]
            tm = lgt[:, 3, :]
            I('dve', 'tensor_tensor', lg, pL[:, 0:NE], brouter[:], ALU.add)
            I('dve', 'max', m8[:, 0:8], lg)
            I('dve', 'tensor_scalar', mk, lg, m8[:, 3:4], None, ALU.is_ge)
            I('dve', 'tensor_scalar', m8[:, 8:9], m8[:, 0:1], -1.0, None, ALU.mult)
            act(ex, lg, AF.Exp, bias=m8[:, 8:9], scale=1.0)
            I('dve', 'tensor_tensor', ex, ex, mk, ALU.mult)
            I('dve', 'tensor_reduce', m8[:, 9:10], ex, mybir.AxisListType.X, ALU.add)
            I('dve', 'reciprocal', m8[:, 10:11], m8[:, 9:10])
            I('dve', 'tensor_scalar', Gt[:, i, :], ex, m8[:, 10:11], None, ALU.mult)
            I('dve', 'tensor_copy', maskb[:, i, :], mk)
            I('dve', 'tensor_copy', Ghl[:, i, :, 0], Gt[:, i, :])
            I('dve', 'tensor_tensor', tm, Gt[:, i, :], Ghl[:, i, :, 0], ALU.subtract)
            I('dve', 'tensor_copy', Ghl[:, i, :, 1], tm)
            if _b2cut == 'C':
                continue
            pP = ps[3]
            for ip in range(i):
                mm(pP[:, 0:NE], ones_b[:], maskb[:, ip, :], start=(ip == 0), stop=False)
            mm(pP[:, 0:NE], ustr_b[:], maskb[:, i, :], start=(i == 0), stop=True)
            I('dve', 'scalar_tensor_tensor', posm[:, i, :], pP[:, 0:NE], 1.0, mk, ALU.add, ALU.mult)
            I('dve', 'tensor_scalar', posm[:, i, :], posm[:, i, :], -1.0, None, ALU.add)
            if _b2cut == 'D':
                continue
            I('dve', 'tensor_scalar', acc[:, i, :], acc[:, i, :], ALPHA, None, ALU.mult)
    if debug and stop >= 4:
        dbg['h1b'] = (h1b, [128, 8, D], BF16)
        dbg['Gt'] = (Gt, [128, 8, NE], F32)
        dbg['posm'] = (posm, [128, 8, NE], F32)
        dbg['acc0'] = (acc, [128, 8, D], F32)

    if stop >= 5:
        wring = [sb("wring%d" % i, [128, 16, 512], BF16, (114 + 16 * i) * KB) for i in range(3)]
        xgT = sb("xgT", [128, 16, CAP], BF16, 162 * KB)
        actT = sb("actT", [128, 16, CAP], BF16, 170 * KB)
        y_e = sb("y_e", [128, 2, D], BF16, 178 * KB)
        S_ = sb("S_", [128, 8, CAP], BF16, 186 * KB)
        ST = [sb("ST%d" % i, [128, 2, NOWN], BF16, (190 + 4 * i) * KB) for i in range(2)]
        tg = [sb("tg%d" % i, [128, CAP], F32, 198 * KB + i * 1024) for i in range(2)]
        ts_ = [sb("ts%d" % i, [128, CAP], BF16, 200 * KB + i * 512) for i in range(2)]
        tgb = [sb("tgb%d" % i, [128, CAP], BF16, 201 * KB + i * 512) for i in range(4)]
        tu = [sb("tu%d" % i, [128, CAP], F32, 203 * KB + i * 1024) for i in range(2)]
        b2row = [sb("b2row%d" % i, [1, 512], BF16, 205 * KB + i * 1024) for i in range(2)]
        ne_run = NE if stop >= 6 else 2

        loads = []
        for e_ in range(ne_run):
            for k4_ in range(4):
                loads.append(lambda s_, e=e_, k=k4_: [(s_[:], T.w1[e][:, k * 512:(k + 1) * 512].rearrange("(kc p) n -> p kc n", p=128))])
                loads.append(lambda s_, e=e_, k=k4_: [(s_[:], T.w1[e][:, DFF + k * 512:DFF + (k + 1) * 512].rearrange("(kc p) n -> p kc n", p=128))])
            for nb_ in range(4):
                loads.append(lambda s_, e=e_, n=nb_: [(s_[:], T.w2[e][:, n * 512:(n + 1) * 512].rearrange("(kc p) n -> p kc n", p=128))])
        issued = [0]

        def need(i):
            upto = min(i + 2, len(loads) - 1)
            while issued[0] <= upto:
                j = issued[0]
                dmas('pool', loads[j](wring[j % 3]), 'wring%d' % (j % 3))
                issued[0] += 1
            return wring[i % 3]

        def build_S(e):
            for i in range(8):
                I('dve', 'tensor_scalar', S_[:, i, :], iota_j[:], posm[:, i, e:e + 1], None, ALU.is_equal)

        def gather(e):
            for dp in range(8):
                pX = ps[6 + dp % 2]
                for half in range(2):
                    dc = 2 * dp + half
                    for i in range(8):
                        mm(pX[:, half * CAP:(half + 1) * CAP], h1b[:, i, dc * 128:(dc + 1) * 128], S_[:, i, :], start=(i == 0), stop=(i == 7))
                if dp % 2 == 0:
                    act(xgT[:, 2 * dp:2 * dp + 2, :], pX[:, :].rearrange("p (c t) -> p c t", c=2), AF.Copy)
                else:
                    I('dve', 'tensor_copy', xgT[:, 2 * dp:2 * dp + 2, :], pX[:, :].rearrange("p (c t) -> p c t", c=2))
            pg = ps[2]
            for jc in range(2):
                for i in range(8):
                    mm(pg[:, 2 * jc:2 * jc + 2], S_[:, i, jc * 128:(jc + 1) * 128], Ghl[:, i, e, :], start=(i == 0), stop=(i == 7))
            gg = gate_g[:, e % 2, :]
            I('dve', 'tensor_copy', smallf[:, 200:204], pg[:, 0:4])
            I('dve', 'tensor_tensor', gg, smallf[:, 200:204:2], smallf[:, 201:204:2], ALU.add)
            st = ST[e % 2]
            for jc in range(2):
                pT = ps[4 + jc][:].bitcast(BF16)
                for i in range(8):
                    tp(pT[:, i * 128:(i + 1) * 128], S_[:, i, jc * 128:(jc + 1) * 128], ident_b[:])
                act(st[:, jc, :], pT[:, 0:NOWN], AF.Copy)

        def mm1(e):
            for k4 in range(4):
                sg = need(12 * e + 2 * k4)
                for cc in range(4):
                    pz = ps[cc // 2][:, (cc % 2) * CAP:(cc % 2 + 1) * CAP]
                    for kc in range(16):
                        mm(pz, sg[:, kc, cc * 128:(cc + 1) * 128], xgT[:, kc, :], start=(kc == 0), stop=(kc == 15))
                for cc in range(4):
                    c = 4 * k4 + cc
                    pz = ps[cc // 2][:, (cc % 2) * CAP:(cc % 2 + 1) * CAP]
                    I('dve', 'tensor_scalar', tg[c % 2][:, :], pz, b1_cols[:, e, c:c + 1], 7.0, ALU.add, ALU.min)
                    act(ts_[c % 2][:, :], tg[c % 2][:, :], AF.Sigmoid, scale=1.702)
                    I('pool', 'tensor_tensor', tgb[cc][:, :], tg[c % 2][:, :], ts_[c % 2][:, :], ALU.mult)
                su = need(12 * e + 2 * k4 + 1)
                for cc in range(4):
                    pz = ps[2 + cc // 2][:, (cc % 2) * CAP:(cc % 2 + 1) * CAP]
                    for kc in range(16):
                        mm(pz, su[:, kc, cc * 128:(cc + 1) * 128], xgT[:, kc, :], start=(kc == 0), stop=(kc == 15))
                for cc in range(4):
                    c = 4 * k4 + cc
                    pz = ps[2 + cc // 2][:, (cc % 2) * CAP:(cc % 2 + 1) * CAP]
                    u_ = tu[c % 2]
                    I('dve', 'tensor_scalar', u_[:, :], pz, b1_cols[:, e, 16 + c:17 + c], 7.0, ALU.add, ALU.min)
                    I('dve', 'tensor_scalar', u_[:, :], u_[:, :], -7.0, 1.0, ALU.max, ALU.add)
                    I('pool', 'tensor_tensor', actT[:, c, :], tgb[cc][:, :], u_[:, :], ALU.mult)

        def mm2(e):
            for nb in range(4):
                slot = need(12 * e + 8 + nb)
                br = b2row[nb % 2]
                dma('pool', br[0:1, :], T.b2_d[e:e + 1, nb * 512:(nb + 1) * 512], 'b2r%d' % (nb % 2))
                for jt in range(2):
                    pY = ps[2 + jt]
                    for kc in range(16):
                        mm(pY[:, :], actT[:, kc, jt * 128:(jt + 1) * 128], slot[:, kc, :], start=(kc == 0), stop=False)
                    mm(pY[:, :], ones_b[0:1, :], br[0:1, :], start=False, stop=True)
                    act(y_e[:, jt, nb * 512:(nb + 1) * 512], pY[:, :], AF.Copy, scale=gate_g[:, e % 2, jt:jt + 1])

        def scatter(e):
            st = ST[e % 2]
            k = 0
            for i in range(8):
                for nb in range(4):
                    pC = ps[4 + k % 2]
                    k += 1
                    mm(pC[:, :], st[:, 0, i * 128:(i + 1) * 128], y_e[:, 0, nb * 512:(nb + 1) * 512], start=True, stop=False)
                    mm(pC[:, :], st[:, 1, i * 128:(i + 1) * 128], y_e[:, 1, nb * 512:(nb + 1) * 512], start=False, stop=True)
                    I('dve', 'tensor_tensor', acc[:, i, nb * 512:(nb + 1) * 512], acc[:, i, nb * 512:(nb + 1) * 512], pC[:, :], ALU.add)

        _mc = _os.environ.get('MOECUT', '9')
        need(0)
        build_S(0)
        gather(0)
        for e in range(ne_run if _mc == '9' else 1):
            if _mc >= '2':
                mm1(e)
            if e + 1 < ne_run and _mc == '9':
                build_S(e + 1)
                gather(e + 1)
            if _mc >= '3':
                mm2(e)
            if _mc >= '4':
                scatter(e)

    if stop >= 4:
        lng2 = sb("lng2", [128, D], F32, 114 * KB)
        lnb2 = sb("lnb2", [128, D], F32, 122 * KB)
        dmas('sp', [(lng2[:], T.ln2_bc_d[0]), (lnb2[:], T.ln2_bc_d[1])], 'lnbc2')
        outs = []
        for i in range(8):
            a_ = acc[:, i, :]
            rstd, nmr = ln_stats(a_, 128, LN_EPS)
            act(a_, a_, AF.Identity, bias=nmr, scale=rstd)
            I('dve', 'tensor_tensor', a_, a_, lng2[:], ALU.mult)
            I('pool', 'tensor_tensor', a_, a_, lnb2[:], ALU.add)
            outs.append(dma('sp', out_d[i * 128:(i + 1) * 128, :], a_, 'out%d' % (i % 4)))
    else:
        outs = []
        outs.append(dma('sp', out_d[0:128, 0:256], smallf[:, 0:256], 'out0'))

    for name, (h, shape, dt) in dbg.items():
        dd = nc.dram_tensor("dbg_" + name, list(shape), dt, kind="ExternalOutput").ap()
        outs.append(dma('sp', dd, h[:], 'dbg'))

    fin = P.op('sp', lambda e: e.nop(), r=(), w=())
    P.ops[fin]['deps'] = set(outs)
    P.emit()
    nc._used_inputs = list(used_inputs)
    return nc


def _perm(s):
    return [0, 1, 2, 3] if s == 1 else [1, 0, 3, 2]


def _rope_tables():
    inv_freq = (1.0 / (10000.0 ** (np.arange(0, 64, 2, dtype=np.float32) / np.float32(64)))).astype(np.float32)
    pos = np.arange(LK, dtype=np.float32)
    freqs = (pos[:, None] * inv_freq[None, :]).astype(np.float32)
    emb = np.concatenate([freqs, freqs], axis=-1)
    cos = np.cos(emb).astype(np.float32)
    sin = np.sin(emb).astype(np.float32)
    sign = np.concatenate([-np.ones(32, np.float32), np.ones(32, np.float32)])
    return cos, sin * sign[None, :]


def make_in_maps(inputs, ne_alloc=NE):
    f = lambda a: np.ascontiguousarray(np.asarray(a, dtype=np.float32))
    x = f(inputs["x"])
    meta = f(inputs["meta_tokens"])
    cos, sinS = _rope_tables()
    shared = {}
    shared["ident"] = np.eye(128, dtype=np.float32)
    shared["tri"] = np.triu(np.ones((128, 128), np.float32))
    shared["ustrict"] = np.triu(np.ones((128, 128), np.float32), 1)
    shared["iota"] = np.ascontiguousarray(np.broadcast_to(np.arange(CAP, dtype=np.float32)[None, :], (128, CAP)))
    shared["w_in"] = f(inputs["w_in"][0])
    shared["w_uq"] = f(inputs["w_uq"][0])
    shared["w_uk"] = f(inputs["w_uk"][0])
    shared["w_uv"] = f(inputs["w_uv"][0])
    shared["w_out"] = f(inputs["w_out"][0])
    shared["w_router"] = f(inputs["w_router"][0])
    shared["w1"] = f(inputs["w_mlp1"][0][:ne_alloc])
    shared["w2"] = f(inputs["w_mlp2"][0][:ne_alloc])
    cols = lambda v, n: np.ascontiguousarray(f(v).reshape(n, 128).T)
    bc = lambda g, b: np.ascontiguousarray(np.stack([np.broadcast_to(f(g)[None, :], (128, D)), np.broadcast_to(f(b)[None, :], (128, D))]))
    shared["lnin_cols"] = np.ascontiguousarray(np.concatenate([cols(inputs["ln_in_g"], 16), cols(inputs["ln_in_b"], 16)], axis=1))
    shared["lnin_bc"] = bc(inputs["ln_in_g"], inputs["ln_in_b"])
    shared["ln1_bc"] = bc(inputs["ln1_g"][0], inputs["ln1_b"][0])
    shared["ln2_bc"] = bc(inputs["ln2_g"][0], inputs["ln2_b"][0])
    shared["qg_col"] = cols(inputs["q_norm_g"][0], 6)
    shared["kvg_col"] = cols(inputs["kv_norm_g"][0], 4)
    dw = f(inputs["conv_dw_w"][0])
    shared["dw_cols"] = np.ascontiguousarray(dw.reshape(31, 8, 128).transpose(2, 1, 0))
    shared["dwb_col"] = cols(inputs["conv_dw_b"][0], 8)
    shared["cln_cols"] = np.ascontiguousarray(np.concatenate([cols(inputs["conv_ln_g"][0], 8), cols(inputs["conv_ln_b"][0], 8)], axis=1))
    shared["b1_cols"] = np.ascontiguousarray(f(inputs["b_mlp1"][0]).reshape(NE, 32, 128).transpose(2, 0, 1))
    shared["b2"] = f(inputs["b_mlp2"][0])
    shared["brouter_bc"] = np.ascontiguousarray(np.broadcast_to(f(inputs["b_router"][0])[None, :], (128, NE)))
    in_maps = []
    for core in range(8):
        b, s = core // 2, core % 2
        perm = _perm(s)
        xb = x[b]
        rows = np.concatenate([xb[512 * tb:512 * (tb + 1)] for tb in perm] + [meta, np.zeros((112, D), np.float32)], axis=0)
        seqpos = np.concatenate([16 + 512 * tb + np.arange(512) for tb in perm] + [np.arange(16)])
        m = dict(shared)
        m["xall"] = np.ascontiguousarray(rows)
        pad = np.zeros((64, LKP - LK), np.float32)
        m["cosT"] = np.ascontiguousarray(np.concatenate([cos[seqpos].T, pad], axis=1))
        m["sinT"] = np.ascontiguousarray(np.concatenate([sinS[seqpos].T, pad], axis=1))
        kbias = np.zeros((2, LKP), np.float32)
        kbias[:, LK:] = NEG
        for slot, qpb in ((0, 1), (1, 2)):
            for pb in range(4):
                if pb != qpb and not (perm[pb] < perm[qpb]):
                    kbias[slot, 512 * pb:512 * (pb + 1)] = NEG
        m["kbias"] = kbias
        halo = np.zeros((128, D), np.float32)
        hm = np.zeros((64,), np.float32)
        seq = np.concatenate([meta, xb], axis=0)
        for blk, qpb in enumerate((1, 2)):
            t0 = 16 + 512 * perm[qpb]
            for j in range(32):
                sp_ = t0 - 32 + j
                if sp_ >= 0:
                    halo[32 * blk + j] = seq[sp_]
                    hm[32 * blk + j] = 1.0
        m["xhalo"] = halo
        m["halomask"] = np.ascontiguousarray(np.broadcast_to(hm[None, :], (128, 64)))
        in_maps.append(m)
    return in_maps


def assemble(results):
    out = np.zeros((4, SEQ, D), np.float32)
    for core in range(8):
        b, s = core // 2, core % 2
        perm = _perm(s)
        o = np.asarray(results[core]["out"], dtype=np.float32)
        out[b, 512 * perm[1]:512 * (perm[1] + 1)] = o[0:512]
        out[b, 512 * perm[2]:512 * (perm[2] + 1)] = o[512:1024]
    return out


def kernel(**inputs):
    nc = build_nc()
    in_maps = make_in_maps(inputs)
    in_maps = [{k: m[k] for k in nc._used_inputs} for m in in_maps]
    res = run_bass_kernel_spmd(nc, in_maps, core_ids=list(range(8)))
    return assemble(res.results)
```
